# Optimizing a Trainium2 kernel written in Bass

```python
import math
import jax, jax.numpy as jnp
from jax import lax
import numpy as np

D_MODEL = 2048
BATCH = 2
SEQ = 4096
DEPTH = 1

ATT_HEADS = 8
ATT_QK_DIM = 64
ATT_V_DIM = 2 * ATT_QK_DIM
ATT_WIDTH = ATT_HEADS * ATT_V_DIM
ROPE_DIM = ATT_QK_DIM // 4
ROPE_THETA = 500000.0
Q_BLOCK = 128
RWKV_HEAD = 64
RWKV_WIDTH = D_MODEL // 2
RWKV_HEADS = RWKV_WIDTH // RWKV_HEAD
DECAY_RANK = 64
ICLR_RANK = 64
GATE_RANK = 128
QK_COLS = ATT_HEADS * 2 * ATT_QK_DIM
ATT_COLS = 2 * QK_COLS + ATT_WIDTH
RWKV_COLS = 3 * RWKV_WIDTH + DECAY_RANK + ICLR_RANK + GATE_RANK
GATE_COLS = 2 * D_MODEL
IN_COLS = ATT_COLS + RWKV_COLS + GATE_COLS
N_GROUPS = 8
EXPERTS_PER_GROUP = 8
N_EXPERTS = N_GROUPS * EXPERTS_PER_GROUP
EXPERT_FF = D_MODEL // 2
TOP_K = 2
EXPERT_BLOCK = 128
NORM_EPS = 1e-6
SUBLN_EPS = 1e-5
LNX_EPS = 64e-5

kernel_name = "hybrid_diffattn_rwkv7_hmoe"


def rmsnorm(x, g, eps=NORM_EPS):
    xf = x.astype(jnp.float32)
    y = xf * lax.rsqrt(jnp.mean(xf * xf, axis=-1, keepdims=True) + eps)
    return (y * g).astype(x.dtype)


def rope_partial(t, positions):
    half = ROPE_DIM // 2
    inv = ROPE_THETA ** (-jnp.arange(half, dtype=jnp.float32) / half)
    ang = positions.astype(jnp.float32)[..., None] * inv
    cos = jnp.cos(ang)[:, :, None, None, :]
    sin = jnp.sin(ang)[:, :, None, None, :]
    t1 = t[..., :half].astype(jnp.float32)
    t2 = t[..., half:ROPE_DIM].astype(jnp.float32)
    rot = jnp.concatenate([t1 * cos - t2 * sin, t1 * sin + t2 * cos], axis=-1).astype(t.dtype)
    return jnp.concatenate([rot, t[..., ROPE_DIM:]], axis=-1)


def diff_attention(q, k, v, lam, lambda_init, subln_g):
    B, S, H = v.shape[:3]
    nqb = S // Q_BLOCK
    qb = (q * (ATT_QK_DIM ** -0.5)).reshape(B, nqb, Q_BLOCK, H, 2, ATT_QK_DIM).transpose(1, 0, 3, 4, 2, 5)
    kt = k.transpose(0, 2, 3, 1, 4)
    vt = v.transpose(0, 2, 1, 3)
    kpos = jnp.arange(S)

    def one_block(args):
        qblk, i = args
        s = jnp.einsum('bhcqd,bhckd->bhcqk', qblk, kt, preferred_element_type=jnp.float32)
        qpos = i * Q_BLOCK + jnp.arange(Q_BLOCK)
        s = jnp.where(kpos[None, :] <= qpos[:, None], s, -jnp.inf)
        p = jax.nn.softmax(s, axis=-1)
        a = p[:, :, 0] - lam * p[:, :, 1]
        return jnp.einsum('bhqk,bhkd->bhqd', a.astype(vt.dtype), vt)

    o = lax.map(one_block, (qb, jnp.arange(nqb)))
    o = o.transpose(1, 0, 3, 2, 4).reshape(B, S, H, ATT_V_DIM)
    o = rmsnorm(o, subln_g, SUBLN_EPS) * (1.0 - lambda_init)
    return o.reshape(B, S, ATT_WIDTH)


def rwkv7_timemix(p, mu, w0, w2, a0, a2, g2, k_k, k_a, r_k, lnx_g, lnx_b):
    B, S, _ = p.shape
    f32 = jnp.float32
    prev = jnp.pad(p[:, :-1], ((0, 0), (1, 0), (0, 0)))
    p = p + (prev - p) * mu
    splits = [RWKV_WIDTH, 2 * RWKV_WIDTH, 3 * RWKV_WIDTH, 3 * RWKV_WIDTH + DECAY_RANK,
              3 * RWKV_WIDTH + DECAY_RANK + ICLR_RANK]
    r, k, v, wl, al, gl = jnp.split(p, splits, axis=-1)
    w_log = -jax.nn.softplus(-(w0 + jnp.tanh(wl) @ w2)) - 0.5
    decay = jnp.exp(-jnp.exp(w_log.astype(f32)))
    a = jax.nn.sigmoid(a0 + al @ a2)
    g = jax.nn.sigmoid(gl) @ g2
    heads = lambda t: t.reshape(B, S, RWKV_HEADS, RWKV_HEAD)
    kk = heads(k * k_k).astype(f32)
    kk = kk * lax.rsqrt(jnp.maximum(jnp.sum(kk * kk, axis=-1, keepdims=True), 1e-24))
    k = k * (1.0 + (a - 1.0) * k_a)
    rh, dh, kh, vh, ah = (heads(t).astype(f32) for t in (r, decay, k, v, a))
    xs = tuple(t.transpose(1, 0, 2, 3) for t in (rh, dh, kh, vh, -kk, kk * ah))

    def step(state, inp):
        r_t, w_t, k_t, v_t, a_t, b_t = inp
        sa = jnp.einsum('bhvk,bhk->bhv', state, a_t)
        state = state * w_t[:, :, None, :] + sa[..., None] * b_t[:, :, None, :] + v_t[..., None] * k_t[:, :, None, :]
        return state, jnp.einsum('bhvk,bhk->bhv', state, r_t)

    s0 = jnp.zeros((B, RWKV_HEADS, RWKV_HEAD, RWKV_HEAD), f32)
    _, ys = lax.scan(step, s0, xs)
    y = ys.transpose(1, 0, 2, 3)
    mean = jnp.mean(y, axis=-1, keepdims=True)
    var = jnp.mean(jnp.square(y - mean), axis=-1, keepdims=True)
    y = ((y - mean) * lax.rsqrt(var + LNX_EPS)).reshape(B, S, RWKV_WIDTH) * lnx_g + lnx_b
    bonus = jnp.sum(rh * kh * r_k, axis=-1, keepdims=True) * vh
    y = (y + bonus.reshape(B, S, RWKV_WIDTH)) * g
    return y.astype(p.dtype)


def hier_moe(h, w_rg, b_rg, w_re, b_re, w_gate, w_up, w_down):
    B, S, D = h.shape
    N = B * S
    f32 = jnp.float32
    hf = h.reshape(N, D)
    gprob = jax.nn.softmax((hf @ w_rg).astype(f32) + b_rg, axis=-1)
    gp, gidx = lax.top_k(gprob, 1)
    elog = ((hf @ w_re).astype(f32) + b_re).reshape(N, N_GROUPS, EXPERTS_PER_GROUP)
    elog = jnp.take_along_axis(elog, gidx[:, :, None], axis=1)[:, 0]
    ep, eidx = lax.top_k(jax.nn.softmax(elog, axis=-1), TOP_K)
    ew = (gp * ep / jnp.sum(ep, axis=-1, keepdims=True)).reshape(-1)
    expert_id = (gidx * EXPERTS_PER_GROUP + eidx).reshape(-1).astype(jnp.int32)
    flat_tok = jnp.repeat(jnp.arange(N, dtype=jnp.int32), TOP_K)
    A = N * TOP_K
    nblk = -(-A // EXPERT_BLOCK) + N_EXPERTS
    order = jnp.argsort(expert_id)
    se, stok, sw = expert_id[order], flat_tok[order], ew[order]
    counts = jnp.bincount(expert_id, length=N_EXPERTS).astype(jnp.int32)
    padded = (counts + EXPERT_BLOCK - 1) // EXPERT_BLOCK * EXPERT_BLOCK
    pad_end = jnp.cumsum(padded)
    pad_start = pad_end - padded
    start = jnp.cumsum(counts) - counts
    dest = pad_start[se] + jnp.arange(A, dtype=jnp.int32) - start[se]
    buf_tok = jnp.full((nblk * EXPERT_BLOCK,), N, jnp.int32).at[dest].set(stok)
    buf_w = jnp.zeros((nblk * EXPERT_BLOCK,), f32).at[dest].set(sw)
    blk_expert = jnp.minimum(jnp.searchsorted(pad_end, jnp.arange(nblk, dtype=jnp.int32) * EXPERT_BLOCK, side='right'), N_EXPERTS - 1)
    h_pad = jnp.concatenate([hf, jnp.zeros((1, D), hf.dtype)], axis=0)

    def run_block(args):
        tok, e = args
        xb = h_pad[tok]
        u = jax.nn.silu(xb @ w_gate[e]) * (xb @ w_up[e])
        return u @ w_down[e]

    yb = lax.map(run_block, (buf_tok.reshape(nblk, EXPERT_BLOCK), blk_expert))
    yb = yb.reshape(-1, D).astype(f32) * buf_w[:, None]
    out = jnp.zeros((N + 1, D), f32).at[buf_tok].add(yb)[:N]
    return out.reshape(B, S, D).astype(h.dtype)


def setup_inputs(seed: int = 0) -> dict:
    key = jax.random.key(seed)
    ks = iter(jax.random.split(key, 48))
    L, D = DEPTH, D_MODEL
    nrm = lambda shape, scale: jax.random.normal(next(ks), shape, jnp.float32) * scale
    x = nrm((BATCH, SEQ, D), 1.0)
    c = nrm((BATCH, D), 1.0)
    positions = jax.random.randint(next(ks), (BATCH, 1), 0, 4096, dtype=jnp.int32) + jnp.arange(SEQ, dtype=jnp.int32)[None, :]
    return {
        'x': x, 'c': c, 'positions': positions,
        'ada_w': nrm((L, D, 6 * D), 0.5 * D ** -0.5),
        'ada_b': nrm((L, 6 * D), 0.02),
        'norm_mix_g': 1.0 + nrm((L, D), 0.02),
        'w_in': nrm((L, D, IN_COLS), D ** -0.5),
        'tshift_mu': jax.random.uniform(next(ks), (L, RWKV_COLS), jnp.float32, 0.0, 1.0),
        'lambda_q1': nrm((L, ATT_QK_DIM), 0.1),
        'lambda_k1': nrm((L, ATT_QK_DIM), 0.1),
        'lambda_q2': nrm((L, ATT_QK_DIM), 0.1),
        'lambda_k2': nrm((L, ATT_QK_DIM), 0.1),
        'subln_g': 1.0 + nrm((L, ATT_V_DIM), 0.02),
        'w0': jax.random.uniform(next(ks), (L, RWKV_WIDTH), jnp.float32, -4.0, 0.0),
        'w2': nrm((L, DECAY_RANK, RWKV_WIDTH), 0.1),
        'a0': nrm((L, RWKV_WIDTH), 0.1),
        'a2': nrm((L, ICLR_RANK, RWKV_WIDTH), 0.1),
        'g2': nrm((L, GATE_RANK, RWKV_WIDTH), GATE_RANK ** -0.5),
        'k_k': 0.85 + nrm((L, RWKV_WIDTH), 0.02),
        'k_a': 1.0 + nrm((L, RWKV_WIDTH), 0.02),
        'r_k': nrm((L, RWKV_HEADS, RWKV_HEAD), 0.1),
        'lnx_g': 1.0 + nrm((L, RWKV_WIDTH), 0.02),
        'lnx_b': nrm((L, RWKV_WIDTH), 0.02),
        'w_up_attn': nrm((L, ATT_WIDTH, D), ATT_WIDTH ** -0.5),
        'w_up_rwkv': nrm((L, RWKV_WIDTH, D), RWKV_WIDTH ** -0.5),
        'w_out': nrm((L, D, D), D ** -0.5),
        'norm_ffn_g': 1.0 + nrm((L, D), 0.02),
        'w_route_group': nrm((L, D, N_GROUPS), D ** -0.5),
        'b_route_group': nrm((L, N_GROUPS), 0.01),
        'w_route_expert': nrm((L, D, N_EXPERTS), D ** -0.5),
        'b_route_expert': nrm((L, N_EXPERTS), 0.01),
        'w_gate': nrm((L, N_EXPERTS, D, EXPERT_FF), D ** -0.5),
        'w_up': nrm((L, N_EXPERTS, D, EXPERT_FF), D ** -0.5),
        'w_down': nrm((L, N_EXPERTS, EXPERT_FF, D), EXPERT_FF ** -0.5),
        'final_g': 1.0 + nrm((D,), 0.02),
    }


def reference(x, c, positions, ada_w, ada_b, norm_mix_g, w_in, tshift_mu, lambda_q1, lambda_k1,
              lambda_q2, lambda_k2, subln_g, w0, w2, a0, a2, g2, k_k, k_a, r_k, lnx_g, lnx_b,
              w_up_attn, w_up_rwkv, w_out, norm_ffn_g, w_route_group, b_route_group,
              w_route_expert, b_route_expert, w_gate, w_up, w_down, final_g):
    B, S, D = x.shape
    f32 = jnp.float32
    h = x
    for l in range(DEPTH):
        ada = jax.nn.silu(c) @ ada_w[l] + ada_b[l]
        sh1, sc1, gt1, sh2, sc2, gt2 = [t[:, None, :] for t in jnp.split(ada, 6, axis=-1)]
        u = rmsnorm(h, norm_mix_g[l]) * (1.0 + sc1) + sh1
        proj = u @ w_in[l]
        p_att, p_rwkv, p_gate = jnp.split(proj, [ATT_COLS, ATT_COLS + RWKV_COLS], axis=-1)
        q, k, v = jnp.split(p_att, [QK_COLS, 2 * QK_COLS], axis=-1)
        q = rope_partial(q.reshape(B, S, ATT_HEADS, 2, ATT_QK_DIM), positions)
        k = rope_partial(k.reshape(B, S, ATT_HEADS, 2, ATT_QK_DIM), positions)
        v = v.reshape(B, S, ATT_HEADS, ATT_V_DIM)
        lambda_init = 0.8 - 0.6 * math.exp(-0.3 * l)
        lam = (jnp.exp(jnp.sum(lambda_q1[l] * lambda_k1[l]).astype(f32))
               - jnp.exp(jnp.sum(lambda_q2[l] * lambda_k2[l]).astype(f32)) + lambda_init)
        y_att = diff_attention(q, k, v, lam, lambda_init, subln_g[l])
        y_rwkv = rwkv7_timemix(p_rwkv, tshift_mu[l], w0[l], w2[l], a0[l], a2[l], g2[l],
                               k_k[l], k_a[l], r_k[l], lnx_g[l], lnx_b[l])
        g_att, g_rwkv = jnp.split(jax.nn.sigmoid(p_gate), 2, axis=-1)
        merged = g_att * (y_att @ w_up_attn[l]) + g_rwkv * (y_rwkv @ w_up_rwkv[l])
        h = h + gt1 * (merged @ w_out[l])
        u2 = rmsnorm(h, norm_ffn_g[l]) * (1.0 + sc2) + sh2
        h = h + gt2 * hier_moe(u2, w_route_group[l], b_route_group[l], w_route_expert[l],
                               b_route_expert[l], w_gate[l], w_up[l], w_down[l])
    return rmsnorm(h, final_g)
```

```python
import math
from contextlib import ExitStack
import numpy as np
import concourse.bass as bass
import concourse.mybir as mybir
from concourse.bass_utils import run_bass_kernel_spmd

F32 = mybir.dt.float32
I32 = mybir.dt.int32
U32 = mybir.dt.uint32
AF = mybir.ActivationFunctionType
ALU = mybir.AluOpType
AX = mybir.AxisListType

NCORES = 8
D = 2048
NT = 8192
S = 4096
KC = 16
OWN = 1024
TWO_PI = 2.0 * math.pi
C1 = 6.28125
C2 = TWO_PI - C1
MAGIC = 12582912.0
ENGS = ("pe", "act", "dve", "pool", "sp")


class Prog:
    def __init__(self, nc, es):
        self.nc = nc
        self.es = es
        self.q = {e: [] for e in ENGS}
        self.cnt = {e: 0 for e in ENGS}
        self.sem = {e: es.enter_context(nc.semaphore("sem_" + e)) for e in ENGS}
        self.waited = {e: {} for e in ENGS}
        self.lastw = {}
        self.readers = {}
        self.nds = 6
        self.dsem = {e: [es.enter_context(nc.semaphore("d%s%d" % (e, i))) for i in range(self.nds)]
                     for e in ("sp", "pool", "act")}
        self.dval = {}
        self.drr = {e: 0 for e in ("sp", "pool", "act")}
        self.ccsem = es.enter_context(nc.semaphore("ccsem"))
        self.ccval = 0
        self.outstanding = []

    def _semof(self, tok):
        if tok[0] == "dma":
            return tok[1], tok[2]
        return self.sem[tok[0]], tok[1]

    def _waits(self, eng, deps):
        ws = []
        for tok in deps:
            if tok[0] == "pe" and eng == "pe":
                continue
            sem, val = self._semof(tok)
            key = id(sem)
            if self.waited[eng].get(key, 0) >= val:
                continue
            self.waited[eng][key] = val
            ws.append((sem, val))
        return ws

    def _deps(self, reads, writes):
        deps = []
        for k in reads:
            if k in self.lastw:
                deps.append(self.lastw[k])
        for k in writes:
            if k in self.lastw:
                deps.append(self.lastw[k])
            deps.extend(self.readers.get(k, ()))
        return deps

    def _commit(self, tok, reads, writes):
        for k in reads:
            self.readers.setdefault(k, []).append(tok)
        for k in writes:
            self.lastw[k] = tok
            self.readers[k] = []

    def op(self, eng, fn, reads=(), writes=(), after_prev=False):
        psr = [k for k in reads if isinstance(k, tuple) and k[0] == "ps"]
        if psr:
            reads = [k for k in reads if k not in psr]
            writes = list(writes) + psr
        deps = self._deps(reads, writes)
        ws = self._waits(eng, deps)
        if after_prev and self.cnt[eng] > 0:
            ws = ws + [(self.sem[eng], self.cnt[eng])]
        self.cnt[eng] += 1
        tok = (eng, self.cnt[eng])
        self.q[eng].append((ws, fn, ("inc", self.sem[eng], 1)))
        self._commit(tok, reads, writes)
        return tok

    def dma(self, qeng, fn, reads=(), writes=()):
        deps = self._deps(reads, writes)
        slot = self.drr[qeng] % self.nds
        self.drr[qeng] += 1
        sem = self.dsem[qeng][slot]
        prev = self.dval.get(id(sem), 0)
        if prev > 0:
            deps.append(("dma", sem, prev))
        ws = self._waits(qeng, deps)
        self.dval[id(sem)] = prev + 16
        tok = ("dma", sem, prev + 16)
        self.q[qeng].append((ws, fn, ("inc", sem, 16)))
        self._commit(tok, reads, writes)
        self.outstanding.append(tok)
        return tok

    def collective(self, fn, reads=(), writes=()):
        deps = self._deps(reads, writes)
        ws = self._waits("pool", deps)
        self.ccval += 1
        tok = ("dma", self.ccsem, self.ccval)
        self.q["pool"].append((ws, fn, ("inc1", self.ccsem, 1)))
        self._commit(tok, reads, writes)
        self.outstanding.append(tok)
        return tok

    def barrier(self):
        toks = [(e, self.cnt[e]) for e in ENGS if self.cnt[e] > 0] + list(self.outstanding)
        self.outstanding = []
        for e in ENGS:
            ws = self._waits(e, toks)
            if ws:
                self.q[e].append((ws, None, None))
        self.lastw = {}
        self.readers = {}

    def emit(self):
        nc = self.nc
        block = self.es.enter_context(nc.Block())
        handles = {"pe": block.tensor, "act": block.scalar, "dve": block.vector,
                   "pool": block.gpsimd, "sp": block.sync}

        def mk(eng):
            items = self.q[eng]

            def body(e):
                for ws, fn, inc in items:
                    for sem, val in ws:
                        e.wait_ge(sem, val)
                    if fn is None:
                        continue
                    ins = fn(e)
                    if inc[0] == "inc":
                        ins.then_inc(inc[1], inc[2])
                    else:
                        ins.then_inc(inc[1])
            return body

        for eng in ENGS:
            handles[eng](mk(eng))


def build(debug=0):
    nc = bass.Bass("TRN2", target_bir_lowering=False)
    es = ExitStack()

    def din(name, shape, dt=F32):
        return nc.dram_tensor(name, list(shape), dt, kind="ExternalInput").ap()

    def dscr(name, shape, dt=F32):
        return nc.dram_tensor(name, list(shape), dt).ap()

    import os
    KBLK = int(os.environ.get('KBLK', '16'))
    KBLKA = int(os.environ.get('KBLKA', '24'))
    KSKIP = os.environ.get('KSKIP', '')
    x = din("x", [KBLK * 512, D])
    cT = din("cT", [128, KC, 2])
    ada_w = din("ada_w", [D, 512 * KBLKA])
    ada_b = din("ada_b", [128, 96, 2])
    g1T = din("g1T", [128, KC])
    wst1 = din("wst1", [D, 1024])
    consts = din("consts", [128, 2560])
    mu5_d = din("mu5", [128, 5])
    rwv_d = din("rwv", [128, 8])
    wa2_d = din("wa2", [128, 128])
    g2s_d = din("g2s", [128, 128])
    posi = din("posi", [2, S], I32)
    lamp = din("lamp", [128, 256])
    sublng = din("sublng", [128, 1])
    dbg = None
    if debug:
        dbg = nc.dram_tensor("dbg", [256, NT], F32, kind="ExternalOutput").ap()

    QT = dscr("QT", [128, NT])
    KT = dscr("KT", [128, NT])
    VTM = dscr("VTM", [NT, 128])
    RW = [dscr("RW%d" % i, [128, NT]) for i in range(5)]
    YSRC = nc.dram_tensor("ysrc", [256, NT], F32, kind="ExternalOutput").ap()
    aux = nc.dram_tensor("aux", [128, 224], F32, kind="ExternalOutput").ap()

    P = Prog(nc, es)
    arena = es.enter_context(nc.sbuf_tensor("arena", [128, 47000], F32))
    pers = es.enter_context(nc.sbuf_tensor("pers", [128, 4608], F32))
    psum = es.enter_context(nc.psum_tensor("psum", [128, 8, 512], F32))

    cst = pers[:, 0:2560]
    ident = cst[:, 0:128]
    ones = cst[:, 128:256]
    blockones = cst[:, 256:384]
    rotT = cst[:, 384:512]
    cmask = cst[:, 512:640]
    invf = cst[:, 640:641]
    cmask64 = cst[:, 1024:1536]
    mask1 = cst[0:64, 1536:2048]
    masklow = cst[0:64, 2048:2304]
    ident4 = cst[0:64, 2304:2560]
    adaT = pers[:, 2560:2752].rearrange("p (j b) -> p j b", b=2)
    A1 = pers[:, 2752:2784].rearrange("p (k b) -> p k b", b=2)
    g1s = pers[:, 2784:2800]
    lam_t = pers[:, 2800:2808]
    sg_t = pers[:, 2808:2809]
    small = pers[:, 2816:2944]
    mu5 = pers[:, 2944:2949]
    rwv = pers[:, 2952:2961]
    wa2 = pers[:, 3072:3200]
    g2s = pers[:, 3200:3328]
    STt = pers[:, 3328:3392]
    bsel = pers[:, 3392:3394]
    ownv = pers[:, 3400:3528]
    yidx = pers[:, 3528:3560].bitcast(I32)
    cst2 = pers[:, 3584:4096]
    Lstrict = cst2[:, 0:128]
    iota256 = cst2[:, 128:384]
    erow256 = cst2[:, 384:448]
    brb = pers[:, 4096:4168]

    def ps(b, n=512, p=128):
        return psum[0:p, b, 0:n]

    P.dma("sp", lambda e: e.dma_start(out=cst, in_=consts), writes=["cst"])
    P.dma("sp", lambda e: e.dma_start(out=g1s, in_=g1T), writes=["g1s"])
    P.dma("sp", lambda e: e.dma_start(out=mu5, in_=mu5_d), writes=["mu5"])
    P.dma("sp", lambda e: e.dma_start(out=rwv[:, 0:8], in_=rwv_d), writes=["rwv"])
    P.dma("sp", lambda e: e.dma_start(out=wa2, in_=wa2_d), writes=["wa2"])
    P.dma("sp", lambda e: e.dma_start(out=g2s, in_=g2s_d), writes=["g2s"])
    P.op("dve", lambda e: e.tensor_scalar(out=rwv[:, 8:9], in0=rwv[:, 3:4], scalar1=-1.0, scalar2=1.0, op0=ALU.mult, op1=ALU.add),
         reads=["rwv"], writes=["rwv"])

    a_off = 0
    scT = arena[:, 0:32].rearrange("p (k b) -> p k b", b=2)
    adab = arena[:, 32:224].rearrange("p (j b) -> p j b", b=2)
    awb = [arena[:, 256 + i * 8192: 256 + (i + 1) * 8192].rearrange("p (k n) -> p k n", n=512) for i in range(2)]
    P.dma("sp", lambda e: e.dma_start(out=scT, in_=cT), writes=["scT"])
    P.dma("sp", lambda e: e.dma_start(out=adab, in_=ada_b), writes=["adab"])
    P.op("act", lambda e: e.activation(out=scT, in_=scT, func=AF.Silu), reads=["scT"], writes=["scT"])
    ada_w_v = ada_w.rearrange("(k p) n -> p k n", p=128)
    for blk in range(KBLKA):
        buf = awb[blk % 2]
        P.dma("sp", lambda e, buf=buf, blk=blk: e.dma_start(out=buf, in_=ada_w_v[:, :, blk * 512:(blk + 1) * 512]),
              writes=[("awb", blk % 2)])
        for jj in range(4):
            j = blk * 4 + jj
            for kc in range(KC):
                P.op("pe", lambda e, buf=buf, jj=jj, j=j, kc=kc: e.matmul(
                    psum[:, 0, 2 * j:2 * j + 2], lhsT=buf[:, kc, jj * 128:(jj + 1) * 128], rhs=scT[:, kc, :],
                    start=(kc == 0), stop=(kc == KC - 1)),
                    reads=[("awb", blk % 2), "scT"], writes=[("ps", 0)])
    P.op("dve", lambda e: e.tensor_tensor(out=adaT, in0=psum[:, 0, 0:192].rearrange("p (j b) -> p j b", b=2), in1=adab, op=ALU.add),
         reads=[("ps", 0), "adab"], writes=["adaT"])
    for b in range(2):
        P.op("dve", lambda e, b=b: e.scalar_tensor_tensor(out=A1[:, :, b], in0=adaT[:, 16:32, b], scalar=1.0, in1=g1s,
                                                           op0=ALU.add, op1=ALU.mult),
             reads=["adaT", "g1s"], writes=["A1"])
    B1 = adaT[:, 0:16, :]
    lp = arena[:, 20000:20256]
    P.dma("sp", lambda e: e.dma_start(out=lp, in_=lamp), writes=["lp"])
    P.dma("sp", lambda e: e.dma_start(out=sg_t, in_=sublng), writes=["sg"])
    P.op("dve", lambda e: e.tensor_tensor(out=lp[:, 0:64], in0=lp[:, 0:64], in1=lp[:, 64:128], op=ALU.mult), reads=["lp"], writes=["lp"])
    P.op("dve", lambda e: e.tensor_tensor(out=lp[:, 128:192], in0=lp[:, 128:192], in1=lp[:, 192:256], op=ALU.mult), reads=["lp"], writes=["lp"])
    P.op("dve", lambda e: e.tensor_reduce(out=small[:, 0:1], in_=lp[:, 0:64], axis=AX.X, op=ALU.add), reads=["lp"], writes=["small"])
    P.op("dve", lambda e: e.tensor_reduce(out=small[:, 1:2], in_=lp[:, 128:192], axis=AX.X, op=ALU.add), reads=["lp"], writes=["small"])
    P.op("act", lambda e: e.activation(out=small[:, 2:4], in_=small[:, 0:2], func=AF.Exp), reads=["small"], writes=["small"])
    P.op("dve", lambda e: e.tensor_tensor(out=small[:, 4:5], in0=small[:, 2:3], in1=small[:, 3:4], op=ALU.subtract), reads=["small"], writes=["small"])
    P.op("dve", lambda e: e.tensor_scalar(out=lam_t[:, 1:2], in0=small[:, 4:5], scalar1=0.2, scalar2=-1.0, op0=ALU.add, op1=ALU.mult),
         reads=["small"], writes=["lam"])
    P.op("dve", lambda e: e.tensor_scalar(out=sg_t, in0=sg_t, scalar1=0.8, scalar2=None, op0=ALU.mult), reads=["sg"], writes=["sg"])
    P.barrier()
    if debug == 10:
        P.dma("sp", lambda e: e.dma_start(out=dbg[0:128, 0:192], in_=pers[:, 2560:2752]), writes=["dbg"])
        P.dma("sp", lambda e: e.dma_start(out=dbg[0:128, 192:200], in_=lam_t), writes=["dbg"])
        P.barrier()
        P.emit()
        es.close()
        return nc

    W1 = arena[:, 0:16384].rearrange("p (k n) -> p k n", n=1024)
    xt = [arena[:, 16384 + i * 2048:16384 + (i + 1) * 2048] for i in range(2)]
    xn = arena[:, 20480:22528]
    uT = arena[:, 22528:30720].rearrange("p (k n) -> p k n", n=512)
    rp = [arena[:, 30720 + i * 512:30720 + (i + 1) * 512] for i in range(10)]
    posb = arena[:, 35840:36352].bitcast(I32)
    stg = [arena[:, 36352 + i * 512:36352 + (i + 1) * 512] for i in range(4)]
    st4 = arena[:, 38400:38416]
    if 'w' not in KSKIP:
        P.dma("sp", lambda e: e.dma_start(out=W1, in_=wst1.rearrange("(k p) n -> p k n", p=128)), writes=["W1"])

    def norm_tile(xsrc_ap, ti, b_of_tile, dst_cols):
        xb = xt[ti % 2]
        kx = ("xt", ti % 2)
        P.dma("sp", lambda e: e.dma_start(out=xb, in_=xsrc_ap), writes=[kx])
        P.op("act", lambda e: e.activation(out=xn, in_=xb, func=AF.Square, accum_out=st4[:, 0:1]),
             reads=[kx], writes=["xn", "st4"])
        P.op("dve", lambda e: e.tensor_scalar(out=st4[:, 1:2], in0=st4[:, 0:1], scalar1=1.0 / D, scalar2=1e-6, op0=ALU.mult, op1=ALU.add),
             reads=["st4"], writes=["st4b"])
        P.op("act", lambda e: e.activation(out=st4[:, 2:3], in_=st4[:, 1:2], func=AF.Sqrt), reads=["st4b"], writes=["st4c"])
        P.op("dve", lambda e: e.reciprocal(out=st4[:, 3:4], in_=st4[:, 2:3]), reads=["st4c"], writes=["st4d"])
        P.op("act", lambda e: e.activation(out=xn, in_=xb, func=AF.Copy, scale=st4[:, 3:4]), reads=[kx, "st4d"], writes=["xn"])
        if 't' in KSKIP:
            return
        for kc in range(KC):
            bank = 4 + kc // 4
            P.op("pe", lambda e, kc=kc, bank=bank: e.transpose(psum[:, bank, (kc % 4) * 128:(kc % 4 + 1) * 128],
                                                                xn[:, kc * 128:(kc + 1) * 128], ident),
                 reads=["xn", "cst"], writes=[("ps", bank)])
        if 'e' in KSKIP:
            return
        for kc in range(KC):
            bank = 4 + kc // 4
            src = psum[:, bank, (kc % 4) * 128:(kc % 4 + 1) * 128]
            dst = uT[:, kc, dst_cols]
            if (kc % 2 == 0 and 'd' not in KSKIP) or 'a' in KSKIP:
                P.op("dve", lambda e, src=src, dst=dst, kc=kc: e.tensor_scalar(
                    out=dst, in0=src, scalar1=A1[:, kc, b_of_tile:b_of_tile + 1], scalar2=B1[:, kc, b_of_tile:b_of_tile + 1],
                    op0=ALU.mult, op1=ALU.add), reads=[("ps", bank), "A1", "adaT"], writes=[("uT", kc)])
            else:
                P.op("act", lambda e, src=src, dst=dst, kc=kc: e.activation(
                    out=dst, in_=src, func=AF.Identity, scale=A1[:, kc, b_of_tile:b_of_tile + 1],
                    bias=B1[:, kc, b_of_tile:b_of_tile + 1]), reads=[("ps", bank), "A1", "adaT"], writes=[("uT", kc)])

    def rope_tables(b, s0):
        P.dma("sp", lambda e: e.dma_start(out=posb, in_=posi[b:b + 1, s0:s0 + 512].partition_broadcast(128)), writes=["posb"])
        P.op("dve", lambda e: e.tensor_copy(out=rp[2], in_=posb), reads=["posb"], writes=["rp2"])
        P.op("dve", lambda e: e.tensor_scalar(out=rp[2], in0=rp[2], scalar1=invf, scalar2=None, op0=ALU.mult),
             reads=["rp2", "cst"], writes=["rp2"])
        for which, shift in ((1, 0.0), (0, 0.25)):
            t, k_, r1 = rp[3], rp[4], rp[5]
            P.op("dve", lambda e, shift=shift: e.tensor_scalar(out=t, in0=rp[2], scalar1=1.0 / TWO_PI, scalar2=shift,
                                                                op0=ALU.mult, op1=ALU.add), reads=["rp2"], writes=["rp3"])
            P.op("dve", lambda e: e.tensor_scalar(out=t, in0=t, scalar1=MAGIC, scalar2=None, op0=ALU.add), reads=["rp3"], writes=["rp3"])
            P.op("dve", lambda e: e.tensor_scalar(out=k_, in0=t, scalar1=-MAGIC, scalar2=None, op0=ALU.add), reads=["rp3"], writes=["rp4"])
            P.op("dve", lambda e: e.scalar_tensor_tensor(out=r1, in0=k_, scalar=-C1, in1=rp[2], op0=ALU.mult, op1=ALU.add),
                 reads=["rp4", "rp2"], writes=["rp5"])
            P.op("dve", lambda e: e.scalar_tensor_tensor(out=r1, in0=k_, scalar=-C2, in1=r1, op0=ALU.mult, op1=ALU.add),
                 reads=["rp4", "rp5"], writes=["rp5"])
            P.op("dve", lambda e, shift=shift: e.tensor_scalar(out=r1, in0=r1, scalar1=shift * TWO_PI, scalar2=math.pi - 1e-6,
                                                                op0=ALU.add, op1=ALU.min), reads=["rp5"], writes=["rp5"])
            P.op("dve", lambda e: e.tensor_scalar(out=r1, in0=r1, scalar1=-(math.pi - 1e-6), scalar2=None, op0=ALU.max),
                 reads=["rp5"], writes=["rp5"])
            P.op("act", lambda e, which=which: e.activation(out=rp[which], in_=r1, func=AF.Sin), reads=["rp5"], writes=[("rp", which)])

    for blk in range(KBLK):
        b = blk // 8
        s0 = (blk % 8) * 512
        for tt in range(4 if 'n' not in KSKIP else 0):
            ti = blk * 4 + tt
            norm_tile(x[ti * 128:(ti + 1) * 128, :], ti, b, slice(tt * 128, (tt + 1) * 128))
        if 'r' not in KSKIP:
            rope_tables(b, s0)
        for fm in range(0 if 'm' not in KSKIP else 7, 7):
            col0 = [0, 128, 384, 512, 640, 768, 896][fm]
            bank = fm % 2
            for kc in range(KC):
                P.op("pe", lambda e, kc=kc, col0=col0, bank=bank: e.matmul(
                    psum[:, bank, :], lhsT=W1[:, kc, col0:col0 + 128], rhs=uT[:, kc, :], start=(kc == 0), stop=(kc == KC - 1)),
                    reads=["W1", ("uT", kc)], writes=[("ps", bank)])
            sg = stg[fm % 2]
            ksg = ("stg", fm % 2)
            if fm < 2:
                qraw = stg[2]
                P.op("act", lambda e, bank=bank: e.activation(out=qraw, in_=psum[:, bank, :], func=AF.Copy),
                     reads=[("ps", bank)], writes=[("stg", 2)])
                P.op("pe", lambda e: e.matmul(psum[:, 2, :], lhsT=rotT, rhs=qraw, start=True, stop=True),
                     reads=[("stg", 2), "cst"], writes=[("ps", 2)])
                P.op("dve", lambda e: e.tensor_tensor(out=stg[3], in0=psum[:, 2, :], in1=rp[1], op=ALU.mult),
                     reads=[("ps", 2), ("rp", 1)], writes=[("stg", 3)])
                P.op("dve", lambda e, sg=sg: e.tensor_tensor(out=sg, in0=qraw, in1=rp[0], op=ALU.mult),
                     reads=[("stg", 2), ("rp", 0)], writes=[ksg])
                P.op("dve", lambda e, sg=sg: e.tensor_tensor(out=sg, in0=sg, in1=stg[3], op=ALU.add),
                     reads=[ksg, ("stg", 3)], writes=[ksg])
                dst = (QT, KT)[fm]
            else:
                P.op("act", lambda e, bank=bank, sg=sg: e.activation(out=sg, in_=psum[:, bank, :], func=AF.Copy),
                     reads=[("ps", bank)], writes=[ksg])
                dst = RW[fm - 2]
            P.dma("pool", lambda e, dst=dst, sg=sg, blk=blk: e.dma_start(out=dst[:, blk * 512:(blk + 1) * 512], in_=sg),
                  reads=[ksg], writes=[("dram", id(dst))])
        if 'v' in KSKIP:
            continue
        for tt in range(4):
            for kc in range(KC):
                P.op("pe", lambda e, kc=kc, tt=tt: e.matmul(
                    psum[:, 3, tt * 128:(tt + 1) * 128], lhsT=uT[:, kc, tt * 128:(tt + 1) * 128], rhs=W1[:, kc, 256:384],
                    start=(kc == 0), stop=(kc == KC - 1)), reads=["W1", ("uT", kc)], writes=[("ps", 3)])
        P.op("act", lambda e: e.activation(out=stg[3], in_=psum[:, 3, :], func=AF.Copy), reads=[("ps", 3)], writes=[("stg", 3)])
        P.dma("pool", lambda e, blk=blk: e.dma_start(
            out=VTM[blk * 512:(blk + 1) * 512, :].rearrange("(t p) n -> p t n", p=128),
            in_=stg[3].rearrange("p (t n) -> p t n", n=128)), reads=[("stg", 3)], writes=[("dram", "VTM")])
    P.barrier()
    if debug == 11:
        for i in range(16):
            for j, src in enumerate((QT, KT)):
                t = stg[j]
                P.dma("sp", lambda e, t=t, i=i, src=src: e.dma_start(out=t, in_=src[:, i * 512:(i + 1) * 512]), reads=[("dram", id(src))], writes=[("stg", j)])
                P.dma("sp", lambda e, t=t, i=i, j=j: e.dma_start(out=dbg[j * 128:(j + 1) * 128, i * 512:(i + 1) * 512], in_=t), reads=[("stg", j)], writes=["dbg"])
        P.barrier()
        P.emit()
        es.close()
        return nc

    qt_s = arena[:, 0:4096]
    kt_s = arena[:, 4096:8192]
    v_s = arena[:, 8192:8192 + 32 * 129].rearrange("p (t n) -> p t n", n=129)
    E_s = [arena[:, 12400 + i * 128:12400 + (i + 1) * 128] for i in range(4)]
    o_s = arena[:, 13000:13128]
    o2_s = arena[:, 13128:13256]
    oT_s = [arena[:, 13256 + i * 128:13256 + (i + 1) * 128] for i in range(2)]
    at4 = arena[:, 13600:13616]
    P.op("dve", lambda e: e.memset(arena[:, 8192:8192 + 32 * 129], 1.0), writes=["v_s"])
    for b in range(2 if 'C' not in KSKIP else 0):
        P.dma("sp", lambda e, b=b: e.dma_start(out=qt_s, in_=QT[:, b * S:(b + 1) * S]), writes=["qt_s"])
        P.dma("sp", lambda e, b=b: e.dma_start(out=kt_s, in_=KT[:, b * S:(b + 1) * S]), writes=["kt_s"])
        P.dma("sp", lambda e, b=b: e.dma_start(out=v_s[:, :, 0:128], in_=VTM[b * S:(b + 1) * S, :].rearrange("(t p) n -> p t n", p=128)),
              writes=["v_s"])
        for qi in range(32):
            ob = 4 + 2 * (qi % 2)
            for ki in range(qi + 1):
                for m in range(2):
                    sb = 2 * (ki % 2) + m
                    P.op("pe", lambda e, m=m, ki=ki, qi=qi, sb=sb: e.matmul(
                        psum[:, sb, 0:128], lhsT=kt_s[m * 64:(m + 1) * 64, ki * 128:(ki + 1) * 128],
                        rhs=qt_s[m * 64:(m + 1) * 64, qi * 128:(qi + 1) * 128], start=True, stop=True),
                        reads=["qt_s", "kt_s"], writes=[("ps", sb)])
                    Eb = E_s[sb]
                    P.op("act", lambda e, sb=sb, Eb=Eb: e.activation(out=Eb, in_=psum[:, sb, 0:128], func=AF.Exp, scale=0.125),
                         reads=[("ps", sb)], writes=[("E", sb)])
                    if ki == qi:
                        P.op("dve", lambda e, Eb=Eb: e.tensor_tensor(out=Eb, in0=Eb, in1=cmask, op=ALU.mult),
                             reads=[("E", sb), "cst"], writes=[("E", sb)])
                    P.op("pe", lambda e, m=m, ki=ki, qi=qi, Eb=Eb, ob=ob: e.matmul(
                        psum[:, ob + m, 0:129], lhsT=Eb, rhs=v_s[:, ki, :], start=(ki == 0), stop=(ki == qi)),
                        reads=[("E", sb), "v_s"], writes=[("ps", ob + m)])
            P.op("dve", lambda e, ob=ob: e.reciprocal(out=at4[:, 0:1], in_=psum[:, ob, 128:129]), reads=[("ps", ob)], writes=["at4a"])
            P.op("dve", lambda e, ob=ob: e.reciprocal(out=at4[:, 1:2], in_=psum[:, ob + 1, 128:129]), reads=[("ps", ob + 1)], writes=["at4b"])
            P.op("dve", lambda e: e.tensor_tensor(out=at4[:, 2:3], in0=at4[:, 1:2], in1=lam_t[:, 1:2], op=ALU.mult),
                 reads=["at4b", "lam"], writes=["at4c"])
            P.op("act", lambda e, ob=ob: e.activation(out=o_s, in_=psum[:, ob, 0:128], func=AF.Copy, scale=at4[:, 0:1]),
                 reads=[("ps", ob), "at4a"], writes=["o_s"])
            P.op("dve", lambda e, ob=ob: e.scalar_tensor_tensor(out=o_s, in0=psum[:, ob + 1, 0:128], scalar=at4[:, 2:3], in1=o_s,
                                                                 op0=ALU.mult, op1=ALU.add),
                 reads=[("ps", ob + 1), "at4c", "o_s"], writes=["o_s"])
            P.op("act", lambda e: e.activation(out=o2_s, in_=o_s, func=AF.Square, accum_out=at4[:, 4:5]),
                 reads=["o_s"], writes=["o2_s", "at4e"])
            P.op("dve", lambda e: e.tensor_scalar(out=at4[:, 5:6], in0=at4[:, 4:5], scalar1=1.0 / 128, scalar2=1e-5, op0=ALU.mult, op1=ALU.add),
                 reads=["at4e"], writes=["at4f"])
            P.op("act", lambda e: e.activation(out=at4[:, 6:7], in_=at4[:, 5:6], func=AF.Sqrt), reads=["at4f"], writes=["at4g"])
            P.op("dve", lambda e: e.reciprocal(out=at4[:, 7:8], in_=at4[:, 6:7]), reads=["at4g"], writes=["at4h"])
            P.op("act", lambda e: e.activation(out=o2_s, in_=o_s, func=AF.Copy, scale=at4[:, 7:8]), reads=["o_s", "at4h"], writes=["o2_s"])
            P.op("pe", lambda e: e.transpose(psum[:, 3, 0:128], o2_s, ident), reads=["o2_s", "cst"], writes=[("ps", 3)])
            oT = oT_s[qi % 2]
            P.op("act", lambda e, oT=oT: e.activation(out=oT, in_=psum[:, 3, 0:128], func=AF.Copy, scale=sg_t),
                 reads=[("ps", 3), "sg"], writes=[("oT", qi % 2)])
            c0 = b * S + qi * 128
            P.dma("pool", lambda e, oT=oT, c0=c0: e.dma_start(out=YSRC[0:128, c0:c0 + 128], in_=oT),
                  reads=[("oT", qi % 2)], writes=[("dram", "YSRC")])
    P.barrier()


    if 'D' not in KSKIP:
        EW = -math.exp(-0.5)
        _n = [0]

        def T(n=512):
            o = _n[0]
            _n[0] += n
            return arena[:, o:o + n]
        raw = [T(520) for _ in range(5)]
        xr, xk, xv, xwa, xg = [T() for _ in range(5)]
        t0, t1, t2, t3 = [T() for _ in range(4)]
        a_t, g_t, logw, Lw, eP, eN, ePm = [T() for _ in range(7)]
        kkn, kmod, bvec, Kt, Bt, Bh, Kh, bonus, eE = [T() for _ in range(9)]
        AR = T(1024)
        AR3 = AR.rearrange("p (c n) -> p c n", n=128)
        PCt = T(8)
        Vtmo = _n[0]
        Vtm = T(1024)[0:64, :].rearrange("p (c n) -> p c n", n=128)
        BhTo = _n[0]
        BhT = T(1024)[0:64, :].rearrange("p (c n) -> p c n", n=128)
        KhTo = _n[0]
        KhT = T(1024)[0:64, :].rearrange("p (c n) -> p c n", n=128)
        G1o = _n[0]
        G1 = T(2048)[0:64, :].rearrange("p (c n) -> p c n", n=128)
        G2o = _n[0]
        G2 = T(2048)[0:64, :].rearrange("p (c n) -> p c n", n=128)
        Pmo = _n[0]
        Pm = T(1024)[0:64, :].rearrange("p (c n) -> p c n", n=64)
        Xb = [T(256)[0:64, :] for _ in range(2)]
        Yb = [T(256)[0:64, :] for _ in range(2)]
        Pb = [T(256)[0:64, :] for _ in range(2)]
        Qb = [T(256)[0:64, :] for _ in range(2)]
        R0 = T(128)[0:64, :]
        Ut = T(128)[0:64, :]
        Ytmo = _n[0]
        Ytm = T(1024)[0:64, :].rearrange("p (c n) -> p c n", n=128)
        YT = T()
        v3 = lambda ap: ap.rearrange("p (c n) -> p c n", n=64)

        def dv(fn, reads, writes):
            P.op("dve", fn, reads=reads, writes=writes)

        def ac(fn, reads, writes):
            P.op("act", fn, reads=reads, writes=writes)

        lastbase = {}

        def pe(fn, reads, writes, base=None):
            bank = [k for k in writes if isinstance(k, tuple) and k[0] == "ps"][0][1]
            prev = lastbase.get(bank)
            drain = base is not None and prev is not None and prev != base
            lastbase[bank] = base
            P.op("pe", fn, reads=reads, writes=writes, after_prev=drain)

        for blk in range(KBLK):
            b = blk // 8
            c0 = blk * 512
            if blk % 8 == 0:
                dv(lambda e: e.memset(STt, 0.0), [], ["ST"])
            for i in range(5):
                if blk % 8 == 0:
                    dv(lambda e, i=i: e.memset(raw[i][:, 0:1], 0.0), [], [("raw", i)])
                    P.dma("sp", lambda e, i=i, c0=c0: e.dma_start(out=raw[i][:, 1:513], in_=RW[i][:, c0:c0 + 512]), writes=[("raw", i)])
                else:
                    P.dma("sp", lambda e, i=i, c0=c0: e.dma_start(out=raw[i][:, 0:513], in_=RW[i][:, c0 - 1:c0 + 512]), writes=[("raw", i)])
            for i, xs in enumerate((xr, xk, xv, xwa, xg)):
                dv(lambda e, i=i: e.tensor_tensor(out=t0, in0=raw[i][:, 0:512], in1=raw[i][:, 1:513], op=ALU.subtract), [("raw", i)], ["t0"])
                dv(lambda e, i=i, xs=xs: e.scalar_tensor_tensor(out=xs, in0=t0, scalar=mu5[:, i:i + 1], in1=raw[i][:, 1:513], op0=ALU.mult, op1=ALU.add),
                   ["t0", ("raw", i), "mu5"], [("x", i)])
            ac(lambda e: e.activation(out=xwa[0:64, :], in_=xwa[0:64, :], func=AF.Tanh), [("x", 3)], [("x", 3)])
            pe(lambda e: e.matmul(psum[:, 0, :], lhsT=wa2[0:64, :], rhs=xwa[0:64, :], start=True, stop=True), [("x", 3), "wa2"], [("ps", 0)], base=0)
            pe(lambda e: e.matmul(psum[:, 1, :], lhsT=wa2[64:128, :], rhs=xwa[64:128, :], start=True, stop=True), [("x", 3), "wa2"], [("ps", 1)], base=64)
            ac(lambda e: e.activation(out=logw, in_=psum[:, 0, :], func=AF.Sigmoid, bias=rwv[:, 0:1]), [("ps", 0), "rwv"], ["logw"])
            dv(lambda e: e.tensor_scalar(out=logw, in0=logw, scalar1=EW, scalar2=None, op0=ALU.mult), ["logw"], ["logw"])
            ac(lambda e: e.activation(out=a_t, in_=psum[:, 1, :], func=AF.Sigmoid, bias=rwv[:, 1:2]), [("ps", 1), "rwv"], ["a_t"])
            ac(lambda e: e.activation(out=t1, in_=xg, func=AF.Sigmoid), [("x", 4)], ["t1"])
            pe(lambda e: e.matmul(psum[:, 0, :], lhsT=g2s, rhs=t1, start=True, stop=True), ["t1", "g2s"], [("ps", 0)])
            ac(lambda e: e.activation(out=g_t, in_=psum[:, 0, :], func=AF.Copy), [("ps", 0)], ["g_t"])
            dv(lambda e: e.tensor_scalar(out=t2, in0=xk, scalar1=rwv[:, 2:3], scalar2=None, op0=ALU.mult), [("x", 1), "rwv"], ["t2"])
            dv(lambda e: e.tensor_tensor(out=t3, in0=t2, in1=t2, op=ALU.mult), ["t2"], ["t3"])
            pe(lambda e: e.matmul(psum[:, 1, :], lhsT=blockones, rhs=t3, start=True, stop=True), ["t3", "cst"], [("ps", 1)])
            dv(lambda e: e.tensor_scalar(out=t3, in0=psum[:, 1, :], scalar1=1e-24, scalar2=None, op0=ALU.max), [("ps", 1)], ["t3"])
            ac(lambda e: e.activation(out=t3, in_=t3, func=AF.Sqrt), ["t3"], ["t3"])
            dv(lambda e: e.reciprocal(out=t3, in_=t3), ["t3"], ["t3"])
            dv(lambda e: e.tensor_tensor(out=kkn, in0=t2, in1=t3, op=ALU.mult), ["t2", "t3"], ["kkn"])
            dv(lambda e: e.tensor_scalar(out=t2, in0=a_t, scalar1=rwv[:, 3:4], scalar2=rwv[:, 8:9], op0=ALU.mult, op1=ALU.add), ["a_t", "rwv"], ["t2"])
            dv(lambda e: e.tensor_tensor(out=kmod, in0=xk, in1=t2, op=ALU.mult), [("x", 1), "t2"], ["kmod"])
            dv(lambda e: e.tensor_tensor(out=bvec, in0=kkn, in1=a_t, op=ALU.mult), ["kkn", "a_t"], ["bvec"])
            dv(lambda e: e.scalar_tensor_tensor(out=t2, in0=xr, scalar=rwv[:, 6:7], in1=kmod, op0=ALU.mult, op1=ALU.mult), [("x", 0), "kmod", "rwv"], ["t2"])
            pe(lambda e: e.matmul(psum[:, 0, :], lhsT=blockones, rhs=t2, start=True, stop=True), ["t2", "cst"], [("ps", 0)])
            dv(lambda e: e.tensor_tensor(out=bonus, in0=psum[:, 0, :], in1=xv, op=ALU.mult), [("ps", 0), ("x", 2)], ["bonus"])
            dv(lambda e: e.tensor_tensor_scan(out=Lw, data0=cmask64, data1=logw, initial=0.0, op0=ALU.mult, op1=ALU.add), ["logw", "cst"], ["Lw"])
            ac(lambda e: e.activation(out=eP, in_=Lw, func=AF.Exp), ["Lw"], ["eP"])
            ac(lambda e: e.activation(out=eN, in_=Lw, func=AF.Exp, scale=-1.0), ["Lw"], ["eN"])
            dv(lambda e: e.tensor_tensor(out=t2, in0=Lw, in1=logw, op=ALU.subtract), ["Lw", "logw"], ["t2"])
            ac(lambda e: e.activation(out=ePm, in_=t2, func=AF.Exp), ["t2"], ["ePm"])
            dv(lambda e: e.tensor_copy(out=PCt, in_=v3(eP)[:, :, 63]), ["eP"], ["PC"])
            dv(lambda e: e.scalar_tensor_tensor(out=AR3[:, :, 0:64], in0=v3(kkn), scalar=-1.0, in1=v3(ePm), op0=ALU.mult, op1=ALU.mult), ["kkn", "ePm"], ["AR"])
            dv(lambda e: e.tensor_tensor(out=AR3[:, :, 64:128], in0=v3(xr), in1=v3(eP), op=ALU.mult), [("x", 0), "eP"], ["AR"])
            dv(lambda e: e.tensor_tensor(out=Kt, in0=kmod, in1=eN, op=ALU.mult), ["kmod", "eN"], ["Kt"])
            dv(lambda e: e.tensor_tensor(out=Bt, in0=bvec, in1=eN, op=ALU.mult), ["bvec", "eN"], ["Bt"])
            for ci in range(8):
                dv(lambda e, ci=ci: e.tensor_scalar(out=eE[:, ci * 64:(ci + 1) * 64], in0=eN[:, ci * 64:(ci + 1) * 64], scalar1=PCt[:, ci:ci + 1],
                                                    scalar2=None, op0=ALU.mult), ["eN", "PC"], ["eE"])
            dv(lambda e: e.tensor_tensor(out=Bh, in0=bvec, in1=eE, op=ALU.mult), ["bvec", "eE"], ["Bh"])
            dv(lambda e: e.tensor_tensor(out=Kh, in0=kmod, in1=eE, op=ALU.mult), ["kmod", "eE"], ["Kh"])
            for src, ksrc, dstT, kdst in ((xv, ("x", 2), Vtm, "Vtm"), (Bh, "Bh", BhT, "BhT"), (Kh, "Kh", KhT, "KhT")):
                for half in range(2):
                    for cc in range(4):
                        ci = half * 4 + cc
                        pe(lambda e, src=src, ci=ci, cc=cc: e.transpose(psum[0:64, 2, cc * 128:(cc + 1) * 128], src[:, ci * 64:(ci + 1) * 64], ident),
                           [ksrc, "cst"], [("ps", 2)])
                    ac(lambda e, dstT=dstT, half=half: e.activation(out=dstT[:, half * 4:(half + 1) * 4, :],
                                                                    in_=psum[0:64, 2, :].rearrange("p (c n) -> p c n", n=128), func=AF.Copy),
                       [("ps", 2)], [kdst])
            for grp in range(4):
                for i in range(4):
                    ci = grp * 2 + i // 2
                    h = i % 2
                    hp = slice(h * 64, (h + 1) * 64)
                    cs = slice(ci * 64, (ci + 1) * 64)
                    pe(lambda e, i=i, hp=hp, cs=cs, ci=ci: e.matmul(psum[0:64, 3, i * 128:(i + 1) * 128], lhsT=Bt[hp, cs], rhs=AR3[hp, ci, :], start=True, stop=True),
                       ["Bt", "AR"], [("ps", 3)], base=h * 64)
                    pe(lambda e, i=i, hp=hp, cs=cs, ci=ci: e.matmul(psum[0:64, 4, i * 128:(i + 1) * 128], lhsT=Kt[hp, cs], rhs=AR3[hp, ci, :], start=True, stop=True),
                       ["Kt", "AR"], [("ps", 4)], base=h * 64)
                    pe(lambda e, i=i, hp=hp, cs=cs, ci=ci: e.matmul(psum[0:64, 5, i * 64:(i + 1) * 64], lhsT=AR3[hp, ci, 0:64], rhs=Bt[hp, cs], start=True, stop=True),
                       ["Bt", "AR"], [("ps", 5)], base=h * 64)
                g1v = G1[:, grp * 4:(grp + 1) * 4, :]
                g2v = G2[:, grp * 4:(grp + 1) * 4, :]
                dv(lambda e, g1v=g1v: e.tensor_tensor(out=g1v, in0=psum[0:64, 3, :].rearrange("p (c n) -> p c n", n=128),
                                                      in1=mask1.rearrange("p (c n) -> p c n", n=128), op=ALU.mult), [("ps", 3), "cst"], ["G1"])
                dv(lambda e, g2v=g2v: e.tensor_tensor(out=g2v, in0=psum[0:64, 4, :].rearrange("p (c n) -> p c n", n=128),
                                                      in1=mask1.rearrange("p (c n) -> p c n", n=128), op=ALU.mult), [("ps", 4), "cst"], ["G2"])
                X, Y_, Pc, Qc = Xb[0], Yb[0], Pb[0], Qb[0]
                dv(lambda e, Y_=Y_: e.tensor_tensor(out=Y_, in0=psum[0:64, 5, 0:256], in1=masklow, op=ALU.mult), [("ps", 5), "cst"], [("Y", 0)])
                dv(lambda e, X=X, g1v=g1v: e.tensor_copy(out=X.rearrange("p (c n) -> p c n", n=64), in_=g1v[:, :, 0:64]), ["G1"], [("X", 0)])
                dv(lambda e, X=X, Pc=Pc: e.tensor_tensor(out=Pc, in0=X, in1=ident4, op=ALU.add), [("X", 0), "cst"], [("P", 0)])
                dv(lambda e, Y_=Y_, Qc=Qc: e.tensor_tensor(out=Qc, in0=Y_, in1=ident4, op=ALU.add), [("Y", 0), "cst"], [("Q", 0)])
                for lv in range(5):
                    cur, nxt = lv % 2, (lv + 1) % 2
                    X, Y_, Pc, Qc = Xb[cur], Yb[cur], Pb[cur], Qb[cur]
                    Xn, Yn, Pn, Qn = Xb[nxt], Yb[nxt], Pb[nxt], Qb[nxt]
                    for i in range(4):
                        sl = slice(i * 64, (i + 1) * 64)
                        pe(lambda e, X=X, Y_=Y_, sl=sl: e.matmul(psum[0:64, 5, sl], lhsT=Y_[:, sl], rhs=X[:, sl], start=True, stop=True),
                           [("X", cur), ("Y", cur)], [("ps", 5)], base=0)
                        pe(lambda e, X=X, Y_=Y_, sl=sl, i=i: e.matmul(psum[0:64, 5, 256 + i * 64:256 + (i + 1) * 64], lhsT=X[:, sl], rhs=Y_[:, sl], start=True, stop=True),
                           [("X", cur), ("Y", cur)], [("ps", 5)], base=0)
                    ac(lambda e, Xn=Xn: e.activation(out=Xn, in_=psum[0:64, 5, 0:256], func=AF.Copy), [("ps", 5)], [("X", nxt)])
                    if lv < 4:
                        ac(lambda e, Yn=Yn: e.activation(out=Yn, in_=psum[0:64, 5, 256:512], func=AF.Copy), [("ps", 5)], [("Y", nxt)])
                    for i in range(4):
                        sl = slice(i * 64, (i + 1) * 64)
                        pe(lambda e, Qc=Qc, Xn=Xn, sl=sl: e.matmul(psum[0:64, 6, sl], lhsT=Qc[:, sl], rhs=Xn[:, sl], start=True, stop=True),
                           [("Q", cur), ("X", nxt)], [("ps", 6)], base=0)
                        if lv < 4:
                            pe(lambda e, Qc=Qc, Xn=Xn, sl=sl, i=i: e.matmul(psum[0:64, 6, 256 + i * 64:256 + (i + 1) * 64], lhsT=Xn[:, sl], rhs=Qc[:, sl], start=True, stop=True),
                               [("Q", cur), ("X", nxt)], [("ps", 6)], base=0)
                    if lv < 4:
                        dv(lambda e, Pn=Pn, Pc=Pc: e.tensor_tensor(out=Pn, in0=psum[0:64, 6, 0:256], in1=Pc, op=ALU.add), [("ps", 6), ("P", cur)], [("P", nxt)])
                        dv(lambda e, Qn=Qn, Qc=Qc: e.tensor_tensor(out=Qn, in0=psum[0:64, 6, 256:512], in1=Qc, op=ALU.add), [("ps", 6), ("Q", cur)], [("Q", nxt)])
                    else:
                        dv(lambda e, Pc=Pc, grp=grp: e.tensor_tensor(out=Pm[:, grp * 4:(grp + 1) * 4, :], in0=psum[0:64, 6, 0:256].rearrange("p (c n) -> p c n", n=64),
                                                                   in1=Pc.rearrange("p (c n) -> p c n", n=64), op=ALU.add), [("ps", 6), ("P", cur)], ["Pm"])
            for ci in range(8 if not (debug == 3 and blk == KBLK - 1 and 'Q' in KSKIP) else 1):
                for h in range(2):
                    hp = slice(h * 64, (h + 1) * 64)
                    vs = slice(h * 64, (h + 1) * 64)
                    pe(lambda e, hp=hp, vs=vs, ci=ci: e.matmul(psum[0:64, 7, vs], lhsT=AR3[hp, ci, 0:64], rhs=STt[hp, :], start=True, stop=False),
                       ["AR", "ST"], [("ps", 7)], base=h * 64)
                    pe(lambda e, h=h, vs=vs, ci=ci: e.matmul(psum[0:64, 7, vs], lhsT=G2[:, ci * 2 + h, 0:64], rhs=Vtm[:, ci, vs], start=False, stop=True),
                       ["G2", "Vtm"], [("ps", 7)], base=0)
                ac(lambda e: e.activation(out=R0, in_=psum[0:64, 7, 0:128], func=AF.Copy), [("ps", 7)], ["R0"])
                for h in range(2):
                    vs = slice(h * 64, (h + 1) * 64)
                    pe(lambda e, h=h, vs=vs, ci=ci: e.matmul(psum[0:64, 7, 128 + h * 64:128 + (h + 1) * 64], lhsT=Pm[:, ci * 2 + h, :], rhs=R0[:, vs], start=True, stop=True),
                       ["Pm", "R0"], [("ps", 7)], base=0)
                ac(lambda e: e.activation(out=Ut, in_=psum[0:64, 7, 128:256], func=AF.Copy), [("ps", 7)], ["Ut"])
                for h in range(2):
                    hp = slice(h * 64, (h + 1) * 64)
                    vs = slice(h * 64, (h + 1) * 64)
                    oy = psum[0:64, 7, 256 + h * 64:256 + (h + 1) * 64]
                    pe(lambda e, hp=hp, ci=ci, oy=oy: e.matmul(oy, lhsT=AR3[hp, ci, 64:128], rhs=STt[hp, :], start=True, stop=False), ["AR", "ST"], [("ps", 7)], base=h * 64)
                    pe(lambda e, h=h, vs=vs, ci=ci, oy=oy: e.matmul(oy, lhsT=G1[:, ci * 2 + h, 64:128], rhs=Ut[:, vs], start=False, stop=False), ["G1", "Ut"], [("ps", 7)], base=0)
                    pe(lambda e, h=h, vs=vs, ci=ci, oy=oy: e.matmul(oy, lhsT=G2[:, ci * 2 + h, 64:128], rhs=Vtm[:, ci, vs], start=False, stop=True), ["G2", "Vtm"], [("ps", 7)], base=0)
                ac(lambda e, ci=ci: e.activation(out=Ytm[:, ci, :], in_=psum[0:64, 7, 256:384], func=AF.Copy), [("ps", 7)], ["Ytm"])
                pe(lambda e, ci=ci: e.matmul(psum[:, 1, 0:128], lhsT=BhT[:, ci, :], rhs=Ut, start=True, stop=False), ["BhT", "Ut"], [("ps", 1)], base=0)
                pe(lambda e, ci=ci: e.matmul(psum[:, 1, 0:128], lhsT=KhT[:, ci, :], rhs=Vtm[:, ci, :], start=False, stop=True), ["KhT", "Vtm"], [("ps", 1)], base=0)
                for h in range(2):
                    hp = slice(h * 64, (h + 1) * 64)
                    dv(lambda e, hp=hp, h=h, ci=ci: e.scalar_tensor_tensor(out=STt[hp, :], in0=STt[hp, :], scalar=PCt[hp, ci:ci + 1],
                                                                           in1=psum[hp, 1, h * 64:(h + 1) * 64], op0=ALU.mult, op1=ALU.add),
                       [("ps", 1), "PC", "ST"], ["ST"])
            for ci in range(8):
                pe(lambda e, ci=ci: e.transpose(psum[:, 0, ci * 64:(ci + 1) * 64], Ytm[:, ci, :], ident[0:64, 0:64]), ["Ytm", "cst"], [("ps", 0)], base=0)
            ac(lambda e: e.activation(out=YT, in_=psum[:, 0, :], func=AF.Copy), [("ps", 0)], ["YT"])
            pe(lambda e: e.matmul(psum[:, 1, :], lhsT=blockones, rhs=YT, start=True, stop=True), ["YT", "cst"], [("ps", 1)])
            dv(lambda e: e.scalar_tensor_tensor(out=t0, in0=psum[:, 1, :], scalar=-1.0 / 64, in1=YT, op0=ALU.mult, op1=ALU.add), [("ps", 1), "YT"], ["t0"])
            dv(lambda e: e.tensor_tensor(out=t1, in0=t0, in1=t0, op=ALU.mult), ["t0"], ["t1"])
            pe(lambda e: e.matmul(psum[:, 0, :], lhsT=blockones, rhs=t1, start=True, stop=True), ["t1", "cst"], [("ps", 0)])
            dv(lambda e: e.tensor_scalar(out=t1, in0=psum[:, 0, :], scalar1=1.0 / 64, scalar2=64e-5, op0=ALU.mult, op1=ALU.add), [("ps", 0)], ["t1"])
            ac(lambda e: e.activation(out=t1, in_=t1, func=AF.Sqrt), ["t1"], ["t1"])
            dv(lambda e: e.reciprocal(out=t1, in_=t1), ["t1"], ["t1"])
            dv(lambda e: e.tensor_tensor(out=t0, in0=t0, in1=t1, op=ALU.mult), ["t0", "t1"], ["t0"])
            dv(lambda e: e.tensor_scalar(out=t0, in0=t0, scalar1=rwv[:, 4:5], scalar2=rwv[:, 5:6], op0=ALU.mult, op1=ALU.add), ["t0", "rwv"], ["t0"])
            dv(lambda e: e.tensor_tensor(out=t0, in0=t0, in1=bonus, op=ALU.add), ["t0", "bonus"], ["t0"])
            dv(lambda e: e.tensor_tensor(out=t3, in0=t0, in1=g_t, op=ALU.mult), ["t0", "g_t"], ["t3"])
            P.dma("pool", lambda e, c0=c0: e.dma_start(out=YSRC[128:256, c0:c0 + 512], in_=t3), reads=["t3"], writes=[("dram", "YSRC")])
            if debug == 3 and blk == KBLK - 1:
                P.barrier()
                for j, tl in enumerate((xr, xk, xv, xwa, xg, logw, a_t, g_t, kkn, kmod, bonus, Lw, YT, t3, t0, eP)):
                    P.dma("sp", lambda e, j=j, tl=tl: e.dma_start(out=dbg[0:128, j * 512:(j + 1) * 512], in_=tl), writes=["dbg"])
                P.dma("sp", lambda e: e.dma_start(out=dbg[128:192, 0:2048], in_=arena[0:64, G1o:G1o + 2048]), writes=["dbg"])
                P.dma("sp", lambda e: e.dma_start(out=dbg[128:192, 2048:4096], in_=arena[0:64, G2o:G2o + 2048]), writes=["dbg"])
                P.dma("sp", lambda e: e.dma_start(out=dbg[128:192, 4096:5120], in_=arena[0:64, Pmo:Pmo + 1024]), writes=["dbg"])
                P.dma("sp", lambda e: e.dma_start(out=dbg[128:192, 5120:6144], in_=arena[0:64, Ytmo:Ytmo + 1024]), writes=["dbg"])
                P.dma("sp", lambda e: e.dma_start(out=dbg[128:256, 6144:6208], in_=STt), writes=["dbg"])
                P.dma("sp", lambda e: e.dma_start(out=dbg[128:192, 6208:6336], in_=R0), writes=["dbg"])
                P.dma("sp", lambda e: e.dma_start(out=dbg[128:192, 6336:6464], in_=Ut), writes=["dbg"])
                P.dma("sp", lambda e: e.dma_start(out=dbg[192:256, 0:1024], in_=arena[0:64, Vtmo:Vtmo + 1024]), writes=["dbg"])
                P.dma("sp", lambda e: e.dma_start(out=dbg[192:256, 1024:2048], in_=arena[0:64, BhTo:BhTo + 1024]), writes=["dbg"])
                P.dma("sp", lambda e: e.dma_start(out=dbg[192:256, 2048:3072], in_=arena[0:64, KhTo:KhTo + 1024]), writes=["dbg"])
                P.barrier()
        P.barrier()

    P.dma("sp", lambda e: e.dma_start(out=aux, in_=pers[:, 2560:2784]), writes=["aux"])
    P.barrier()
    if debug == 2:
        for i in range(32):
            t = stg[i % 2]
            r0 = (i // 16) * 128
            cc = (i % 16) * 512
            P.dma("sp", lambda e, t=t, r0=r0, cc=cc: e.dma_start(out=t, in_=YSRC[r0:r0 + 128, cc:cc + 512]), reads=[("dram", "YSRC")], writes=[("stg", i % 2)])
            P.dma("sp", lambda e, t=t, r0=r0, cc=cc: e.dma_start(out=dbg[r0:r0 + 128, cc:cc + 512], in_=t), reads=[("stg", i % 2)], writes=["dbg"])
        P.barrier()
    if debug == 1:
        for i in range(16):
            t = stg[i % 2]
            P.dma("sp", lambda e, t=t, i=i: e.dma_start(out=t, in_=YSRC[0:128, i * 512:(i + 1) * 512]), reads=[("dram", "YSRC")], writes=[("stg", i % 2)])
            P.dma("sp", lambda e, t=t, i=i: e.dma_start(out=dbg[0:128, i * 512:(i + 1) * 512], in_=t), reads=[("stg", i % 2)], writes=["dbg"])
        P.barrier()

    P.emit()
    es.close()
    return nc


def build2(debug=0, nexp=64):
    nc = bass.Bass("TRN2", target_bir_lowering=False)
    es = ExitStack()
    KSKIP = ''
    CAP = 256

    def din(name, shape, dt=F32):
        return nc.dram_tensor(name, list(shape), dt, kind="ExternalInput").ap()

    def dscr(name, shape, dt=F32):
        return nc.dram_tensor(name, list(shape), dt).ap()

    consts = din("consts", [128, 2560])
    c2_d = din("consts2", [128, 512])
    aux_in = din("aux_in", [128, 224])
    yown = din("yown", [16, 128, OWN])
    x_own = din("x_own", [OWN, D])
    bsel_d = din("bsel", [128, 2])
    wupa = din("wupa", [1024, D])
    wupb = din("wupb", [1024, D])
    wgt = din("wgt", [D, 2 * D])
    wout = din("wout", [D, D])
    g2n_d = din("g2n", [128, D])
    fg_d = din("fgb", [128, D])
    wr_d = din("wr", [D, 72])
    br_d = din("br", [128, 72])
    eg_d = din("eg", [nexp * D, 1024])
    eu_d = din("eu", [nexp * D, 1024])
    ed_d = din("ed", [nexp * 1024, D])
    dbg = nc.dram_tensor("dbg", [256, NT], F32, kind="ExternalOutput").ap() if debug else None
    out = nc.dram_tensor("out", [OWN, D], F32, kind="ExternalOutput").ap()
    HSCR = dscr("HSCR", [OWN, D])
    YSL = dscr("YSL", [nexp * CAP, D])

    P = Prog(nc, es)
    arena = es.enter_context(nc.sbuf_tensor("arena", [128, 47000], F32))
    pers = es.enter_context(nc.sbuf_tensor("pers", [128, 4608], F32))
    psum = es.enter_context(nc.psum_tensor("psum", [128, 8, 512], F32))
    cst = pers[:, 0:2560]
    ident = cst[:, 0:128]
    ones = cst[:, 128:256]
    adaT = pers[:, 2560:2752].rearrange("p (j b) -> p j b", b=2)
    A1 = pers[:, 2752:2784].rearrange("p (k b) -> p k b", b=2)
    bsel = pers[:, 3392:3394]
    ownv = pers[:, 3400:3528]
    cst2 = pers[:, 3584:4096]
    Lstrict = cst2[:, 0:128]
    iota256 = cst2[:, 128:384]
    erow256 = cst2[:, 384:448]
    brb = pers[:, 4096:4168]
    P.dma("sp", lambda e: e.dma_start(out=cst, in_=consts), writes=["cst"])
    P.dma("sp", lambda e: e.dma_start(out=pers[:, 2560:2784], in_=aux_in), writes=["adaT", "A1"])
    P.dma("sp", lambda e: e.dma_start(out=bsel, in_=bsel_d), writes=["bsel"])
    P.dma("sp", lambda e: e.dma_start(out=cst2, in_=c2_d), writes=["cst2"])
    P.dma("sp", lambda e: e.dma_start(out=brb, in_=br_d), writes=["brb"])
    srcs = [A1, adaT[:, 0:16, :], adaT[:, 32:48, :], adaT[:, 48:64, :], adaT[:, 64:80, :], adaT[:, 80:96, :]]
    for i, sv in enumerate(srcs):
        dsto = ownv[:, i * 16:(i + 1) * 16]
        P.op("dve", lambda e, sv=sv, dsto=dsto: e.tensor_scalar(out=dsto, in0=sv[:, :, 0], scalar1=bsel[:, 0:1], scalar2=None, op0=ALU.mult),
             reads=["adaT", "A1", "bsel"], writes=["ownv"])
        P.op("dve", lambda e, sv=sv, dsto=dsto: e.scalar_tensor_tensor(out=dsto, in0=sv[:, :, 1], scalar=bsel[:, 1:2], in1=dsto, op0=ALU.mult, op1=ALU.add),
             reads=["adaT", "A1", "bsel", "ownv"], writes=["ownv"])
    A1o, B1o, gt1o, sh2o, sc2o, gt2o = [ownv[:, i * 16:(i + 1) * 16] for i in range(6)]
    o = [0]

    def TA(n):
        r = arena[:, o[0]:o[0] + n]
        o[0] += n
        return r
    yT = TA(8192).rearrange("p (k n) -> p k n", n=512)
    bc = {"gt1": TA(2048)}
    uT2 = TA(8192).rearrange("p (k n) -> p k n", n=512)
    mT = TA(8192).rearrange("p (k n) -> p k n", n=512)
    wA = [TA(1024).rearrange("p (k n) -> p k n", n=128) for _ in range(2)]
    wB = [TA(1024).rearrange("p (k n) -> p k n", n=128) for _ in range(2)]
    wGA = [TA(2048).rearrange("p (k n) -> p k n", n=128) for _ in range(2)]
    wGB = [TA(2048).rearrange("p (k n) -> p k n", n=128) for _ in range(2)]
    xt2 = [TA(2048) for _ in range(2)]
    xn2 = TA(2048)
    dg = TA(128)
    s4 = TA(16)
    sga = TA(512)
    sgb = TA(512)

    def bcast_rows(names):
      for nm, vec in names:
        for kc in range(16):
            P.op("dve", lambda e, vec=vec, kc=kc: e.tensor_scalar(out=dg, in0=ident, scalar1=vec[:, kc:kc + 1], scalar2=None, op0=ALU.mult),
                 reads=["cst", "ownv"], writes=["dg"])
            P.op("pe", lambda e: e.matmul(psum[:, 0, 0:128], lhsT=ones, rhs=dg, start=True, stop=True), reads=["dg", "cst"], writes=[("ps", 0)])
            if nm == "sc2":
                P.op("dve", lambda e, kc=kc: e.scalar_tensor_tensor(out=bc["A2"][:, kc * 128:(kc + 1) * 128], in0=psum[:, 0, 0:128], scalar=1.0,
                                                                  in1=bc["A2"][:, kc * 128:(kc + 1) * 128], op0=ALU.add, op1=ALU.mult),
                     reads=[("ps", 0), "bcA2"], writes=["bcA2"])
            else:
                P.op("act", lambda e, nm=nm, kc=kc: e.activation(out=bc[nm][:, kc * 128:(kc + 1) * 128], in_=psum[:, 0, 0:128], func=AF.Copy),
                     reads=[("ps", 0)], writes=["bc" + nm])
    bcast_rows([("gt1", gt1o)])
    wupa_v = wupa.rearrange("(k p) n -> p k n", p=128)
    wupb_v = wupb.rearrange("(k p) n -> p k n", p=128)
    wgt_v = wgt.rearrange("(k p) n -> p k n", p=128)

    def norm_tile2(xsrc_ap, ti, dst_cols):
        xb = xt2[ti % 2]
        kx = ("xt2", ti % 2)
        P.dma("sp", lambda e: e.dma_start(out=xb, in_=xsrc_ap), writes=[kx])
        P.op("act", lambda e: e.activation(out=xn2, in_=xb, func=AF.Square, accum_out=s4[:, 0:1]), reads=[kx], writes=["xn2", "s4"])
        P.op("dve", lambda e: e.tensor_scalar(out=s4[:, 1:2], in0=s4[:, 0:1], scalar1=1.0 / D, scalar2=1e-6, op0=ALU.mult, op1=ALU.add),
             reads=["s4"], writes=["s4b"])
        P.op("act", lambda e: e.activation(out=s4[:, 2:3], in_=s4[:, 1:2], func=AF.Sqrt), reads=["s4b"], writes=["s4c"])
        P.op("dve", lambda e: e.reciprocal(out=s4[:, 3:4], in_=s4[:, 2:3]), reads=["s4c"], writes=["s4d"])
        P.op("act", lambda e: e.activation(out=xn2, in_=xb, func=AF.Copy, scale=s4[:, 3:4]), reads=[kx, "s4d"], writes=["xn2"])
        for kc in range(KC):
            bank = 4 + kc // 4
            P.op("pe", lambda e, kc=kc, bank=bank: e.transpose(psum[:, bank, (kc % 4) * 128:(kc % 4 + 1) * 128], xn2[:, kc * 128:(kc + 1) * 128], ident),
                 reads=["xn2", "cst"], writes=[("ps", bank)])
        for kc in range(KC):
            bank = 4 + kc // 4
            src = psum[:, bank, (kc % 4) * 128:(kc % 4 + 1) * 128]
            dst = uT2[:, kc, dst_cols]
            P.op("dve" if kc % 2 == 0 else "act",
                 (lambda e, src=src, dst=dst, kc=kc: e.tensor_scalar(out=dst, in0=src, scalar1=A1o[:, kc:kc + 1], scalar2=B1o[:, kc:kc + 1],
                                                                     op0=ALU.mult, op1=ALU.add)) if kc % 2 == 0 else
                 (lambda e, src=src, dst=dst, kc=kc: e.activation(out=dst, in_=src, func=AF.Identity, scale=A1o[:, kc:kc + 1], bias=B1o[:, kc:kc + 1])),
                 reads=[("ps", bank), "ownv"], writes=[("uT2", kc)])

    wo_t = [wGA[i].rearrange("p k n -> p (k n)") for i in range(2)]
    htile = xn2
    for ob in range(2):
        for kc in range(16):
            P.dma("sp", lambda e, kc=kc, ob=ob: e.dma_start(out=yT[:, kc, :], in_=yown[kc, :, ob * 512:(ob + 1) * 512]), writes=[("yT", kc)])
        for tt in range(4):
            ti = ob * 4 + tt
            norm_tile2(x_own[ti * 128:(ti + 1) * 128, :], ti, slice(tt * 128, (tt + 1) * 128))
        tc0 = ob * 512
        for j in range(16):
            pb = j % 2
            P.dma("sp", lambda e, j=j, pb=pb: e.dma_start(out=wA[pb], in_=wupa_v[:, :, j * 128:(j + 1) * 128]), writes=[("wA", pb)])
            P.dma("sp", lambda e, j=j, pb=pb: e.dma_start(out=wB[pb], in_=wupb_v[:, :, j * 128:(j + 1) * 128]), writes=[("wB", pb)])
            P.dma("sp", lambda e, j=j, pb=pb: e.dma_start(out=wGA[pb], in_=wgt_v[:, :, j * 128:(j + 1) * 128]), writes=[("wGA", pb)])
            P.dma("sp", lambda e, j=j, pb=pb: e.dma_start(out=wGB[pb], in_=wgt_v[:, :, D + j * 128:D + (j + 1) * 128]), writes=[("wGB", pb)])
            for kc in range(8):
                P.op("pe", lambda e, kc=kc, pb=pb, tc0=tc0: e.matmul(psum[:, 0, :], lhsT=wA[pb][:, kc, :], rhs=yT[:, kc, :], start=(kc == 0), stop=(kc == 7)),
                     reads=[("wA", pb), ("yT", kc)], writes=[("ps", 0)])
            for kc in range(8):
                P.op("pe", lambda e, kc=kc, pb=pb, tc0=tc0: e.matmul(psum[:, 1, :], lhsT=wB[pb][:, kc, :], rhs=yT[:, 8 + kc, :], start=(kc == 0), stop=(kc == 7)),
                     reads=[("wB", pb), ("yT", 8 + kc)], writes=[("ps", 1)])
            for kc in range(16):
                P.op("pe", lambda e, kc=kc, pb=pb: e.matmul(psum[:, 2, :], lhsT=wGA[pb][:, kc, :], rhs=uT2[:, kc, :], start=(kc == 0), stop=(kc == 15)),
                     reads=[("wGA", pb), ("uT2", kc)], writes=[("ps", 2)])
            for kc in range(16):
                P.op("pe", lambda e, kc=kc, pb=pb: e.matmul(psum[:, 3, :], lhsT=wGB[pb][:, kc, :], rhs=uT2[:, kc, :], start=(kc == 0), stop=(kc == 15)),
                     reads=[("wGB", pb), ("uT2", kc)], writes=[("ps", 3)])
            P.op("act", lambda e: e.activation(out=sga, in_=psum[:, 2, :], func=AF.Sigmoid), reads=[("ps", 2)], writes=["sga"])
            P.op("act", lambda e: e.activation(out=sgb, in_=psum[:, 3, :], func=AF.Sigmoid), reads=[("ps", 3)], writes=["sgb"])
            P.op("dve", lambda e: e.tensor_tensor(out=sga, in0=psum[:, 0, :], in1=sga, op=ALU.mult), reads=[("ps", 0), "sga"], writes=["sga"])
            P.op("dve", lambda e: e.tensor_tensor(out=sgb, in0=psum[:, 1, :], in1=sgb, op=ALU.mult), reads=[("ps", 1), "sgb"], writes=["sgb"])
            P.op("dve", lambda e, j=j: e.tensor_tensor(out=mT[:, j, :], in0=sga, in1=sgb, op=ALU.add), reads=["sga", "sgb"], writes=[("mT", j)])
        for tt in range(4):
            ti = ob * 4 + tt
            for kc in range(16):
                wb = wo_t[kc % 2]
                P.dma("sp", lambda e, kc=kc, wb=wb: e.dma_start(out=wb, in_=wout[kc * 128:(kc + 1) * 128, :]), writes=[("wGA", kc % 2)])
                for cb in range(4):
                    P.op("pe", lambda e, kc=kc, cb=cb, wb=wb, tt=tt: e.matmul(psum[:, 4 + cb, :], lhsT=mT[:, kc, tt * 128:(tt + 1) * 128], rhs=wb[:, cb * 512:(cb + 1) * 512],
                                                                             start=(kc == 0), stop=(kc == 15)),
                         reads=[("wGA", kc % 2), ("mT", kc)], writes=[("ps", 4 + cb)])
            xb = xt2[ti % 2]
            kx = ("xt2", ti % 2)
            P.dma("sp", lambda e, ti=ti, xb=xb: e.dma_start(out=xb, in_=x_own[ti * 128:(ti + 1) * 128, :]), writes=[kx])
            for cb in range(4):
                cs_ = slice(cb * 512, (cb + 1) * 512)
                P.op("dve", lambda e, cb=cb, cs_=cs_: e.tensor_tensor(out=htile[:, cs_], in0=psum[:, 4 + cb, :], in1=bc["gt1"][:, cs_], op=ALU.mult),
                     reads=[("ps", 4 + cb), "bcgt1"], writes=["xn2"])
                P.op("dve", lambda e, cs_=cs_, xb=xb: e.tensor_tensor(out=htile[:, cs_], in0=htile[:, cs_], in1=xb[:, cs_], op=ALU.add),
                     reads=["xn2", kx], writes=["xn2"])
            P.dma("pool", lambda e, ti=ti: e.dma_start(out=HSCR[ti * 128:(ti + 1) * 128, :], in_=htile), reads=["xn2"], writes=[("dram", "HSCR")])
    P.barrier()
    if debug == 4:
        for ti in range(8):
            P.dma("sp", lambda e, ti=ti: e.dma_start(out=htile, in_=HSCR[ti * 128:(ti + 1) * 128, :]), writes=["xn2"])
            P.dma("sp", lambda e, ti=ti: e.dma_start(out=dbg[0:128, ti * 2048:(ti + 1) * 2048] if ti < 4 else dbg[128:256, (ti - 4) * 2048:(ti - 3) * 2048], in_=htile),
                  reads=["xn2"], writes=["dbg"])
        P.barrier()
        P.emit()
        es.close()
        return nc


    o2 = [0]

    def TB(n):
        r = arena[:, o2[0]:o2[0] + n]
        o2[0] += n
        return r
    u2buf = TB(16384).rearrange("p (t n) -> p t n", n=D)
    xbT = TB(16 * CAP).rearrange("p (k n) -> p k n", n=CAP)
    Sel = TB(8 * CAP).rearrange("p (t n) -> p t n", n=CAP)
    h1T = TB(8 * CAP).rearrange("p (f n) -> p f n", n=CAP)
    wgb = [TB(2048).rearrange("p (k n) -> p k n", n=128) for _ in range(2)]
    wub = [TB(2048).rearrange("p (k n) -> p k n", n=128) for _ in range(2)]
    wdb = [TB(4096).rearrange("p (f n) -> p f n", n=512) for _ in range(2)]
    yst = [TB(512) for _ in range(2)]
    OHs = TB(512).rearrange("p (t n) -> p t n", n=64)
    OH1 = TB(512).rearrange("p (t n) -> p t n", n=64)
    OH2 = TB(512).rearrange("p (t n) -> p t n", n=64)
    rank = TB(512).rearrange("p (t n) -> p t n", n=64)
    wk = TB(16).rearrange("p (t n) -> p t n", n=2)
    idxf = TB(16).rearrange("p (t n) -> p t n", n=2)
    idxi = TB(16).bitcast(I32).rearrange("p (t n) -> p t n", n=2)
    rt = TB(256)
    lg = rt[:, 0:72]
    sel = rt[:, 80:88]
    sel2 = rt[:, 88:96]
    ohg = rt[:, 96:104]
    oh1 = rt[:, 104:112]
    oh2 = rt[:, 112:120]
    r1 = rt[:, 120:136]
    tmp64 = rt[:, 136:200]
    bcA2 = TB(0)
    bc["A2"] = wdb[0].rearrange("p f n -> p (f n)")[:, 0:2048]
    bc["sh2"] = wdb[0].rearrange("p f n -> p (f n)")[:, 2048:4096]
    bc["gt2"] = wdb[1].rearrange("p f n -> p (f n)")[:, 0:2048]
    P.dma("sp", lambda e: e.dma_start(out=bc["A2"], in_=g2n_d), writes=["bcA2"])
    bcast_rows([("sh2", sh2o), ("sc2", sc2o)])
    wr_s = wdb[1].rearrange("p f n -> p (f n)")[:, 2048:2048 + 16 * 72].rearrange("p (k n) -> p k n", n=72)
    P.dma("sp", lambda e: e.dma_start(out=wr_s, in_=wr_d.rearrange("(k p) n -> p k n", p=128)), writes=["wr_s"])
    hti = wgb[0].rearrange("p k n -> p (k n)")
    u2T = wub[0].rearrange("p k n -> p (k n)")
    junk = wub[1].rearrange("p k n -> p (k n)")
    for ti in range(8):
        P.dma("sp", lambda e, ti=ti: e.dma_start(out=hti, in_=HSCR[ti * 128:(ti + 1) * 128, :]), reads=[("dram", "HSCR")], writes=["hti"])
        P.op("act", lambda e: e.activation(out=junk, in_=hti, func=AF.Square, accum_out=r1[:, 0:1]), reads=["hti"], writes=["junk", "r1a"])
        P.op("dve", lambda e: e.tensor_scalar(out=r1[:, 1:2], in0=r1[:, 0:1], scalar1=1.0 / D, scalar2=1e-6, op0=ALU.mult, op1=ALU.add), reads=["r1a"], writes=["r1b"])
        P.op("act", lambda e: e.activation(out=r1[:, 2:3], in_=r1[:, 1:2], func=AF.Sqrt), reads=["r1b"], writes=["r1c"])
        P.op("dve", lambda e: e.reciprocal(out=r1[:, 3:4], in_=r1[:, 2:3]), reads=["r1c"], writes=["r1d"])
        u2t = u2buf[:, ti, :]
        P.op("dve", lambda e, u2t=u2t: e.scalar_tensor_tensor(out=u2t, in0=hti, scalar=r1[:, 3:4], in1=bc["A2"], op0=ALU.mult, op1=ALU.mult),
             reads=["hti", "r1d", "bcA2"], writes=[("u2", ti)])
        P.op("dve", lambda e, u2t=u2t: e.tensor_tensor(out=u2t, in0=u2t, in1=bc["sh2"], op=ALU.add), reads=[("u2", ti), "bcsh2"], writes=[("u2", ti)])
        for kc in range(16):
            bank = 4 + kc // 4
            P.op("pe", lambda e, kc=kc, bank=bank, u2t=u2t: e.transpose(psum[:, bank, (kc % 4) * 128:(kc % 4 + 1) * 128], u2t[:, kc * 128:(kc + 1) * 128], ident),
                 reads=[("u2", ti), "cst"], writes=[("ps", bank)])
        for q4 in range(4):
            P.op("act" if q4 % 2 else "dve",
                 (lambda e, q4=q4: e.activation(out=u2T[:, q4 * 512:(q4 + 1) * 512], in_=psum[:, 4 + q4, :], func=AF.Copy)) if q4 % 2 else
                 (lambda e, q4=q4: e.tensor_copy(out=u2T[:, q4 * 512:(q4 + 1) * 512], in_=psum[:, 4 + q4, :])),
                 reads=[("ps", 4 + q4)], writes=[("u2T", q4)])
        for kc in range(16):
            P.op("pe", lambda e, kc=kc: e.matmul(psum[:, 0, 0:72], lhsT=u2T[:, kc * 128:(kc + 1) * 128], rhs=wr_s[:, kc, :], start=(kc == 0), stop=(kc == 15)),
                 reads=[("u2T", kc // 4), "wr_s"], writes=[("ps", 0)])
        dvo = lambda fn, r, w: P.op("dve", fn, reads=r, writes=w)
        dvo(lambda e: e.tensor_tensor(out=lg, in0=psum[:, 0, 0:72], in1=brb, op=ALU.add), [("ps", 0), "brb"], ["lg"])
        dvo(lambda e: e.tensor_reduce(out=r1[:, 4:5], in_=lg[:, 0:8], axis=AX.X, op=ALU.max), ["lg"], ["r1e"])
        dvo(lambda e: e.tensor_scalar(out=ohg, in0=lg[:, 0:8], scalar1=r1[:, 4:5], scalar2=None, op0=ALU.is_equal), ["lg", "r1e"], ["ohg"])
        dvo(lambda e: e.tensor_scalar(out=r1[:, 5:6], in0=r1[:, 4:5], scalar1=-1.0, scalar2=None, op0=ALU.mult), ["r1e"], ["r1f"])
        P.op("act", lambda e: e.activation(out=tmp64[:, 0:8], in_=lg[:, 0:8], func=AF.Exp, bias=r1[:, 5:6], accum_out=r1[:, 6:7]), reads=["lg", "r1f"], writes=["tmp64", "r1g"])
        dvo(lambda e: e.reciprocal(out=r1[:, 7:8], in_=r1[:, 6:7]), ["r1g"], ["r1h"])
        dvo(lambda e: e.tensor_scalar(out=sel, in0=lg[:, 8:16], scalar1=ohg[:, 0:1], scalar2=None, op0=ALU.mult), ["lg", "ohg"], ["sel"])
        for g in range(1, 8):
            dvo(lambda e, g=g: e.scalar_tensor_tensor(out=sel, in0=lg[:, 8 + g * 8:16 + g * 8], scalar=ohg[:, g:g + 1], in1=sel, op0=ALU.mult, op1=ALU.add),
                ["lg", "ohg", "sel"], ["sel"])
        dvo(lambda e: e.tensor_reduce(out=r1[:, 8:9], in_=sel, axis=AX.X, op=ALU.max), ["sel"], ["r1i"])
        dvo(lambda e: e.tensor_scalar(out=oh1, in0=sel, scalar1=r1[:, 8:9], scalar2=None, op0=ALU.is_equal), ["sel", "r1i"], ["oh1"])
        dvo(lambda e: e.scalar_tensor_tensor(out=sel2, in0=oh1, scalar=-1e30, in1=sel, op0=ALU.mult, op1=ALU.add), ["oh1", "sel"], ["sel2"])
        dvo(lambda e: e.tensor_reduce(out=r1[:, 9:10], in_=sel2, axis=AX.X, op=ALU.max), ["sel2"], ["r1j"])
        dvo(lambda e: e.tensor_scalar(out=oh2, in0=sel2, scalar1=r1[:, 9:10], scalar2=None, op0=ALU.is_equal), ["sel2", "r1j"], ["oh2"])
        dvo(lambda e: e.tensor_scalar(out=r1[:, 10:11], in0=r1[:, 8:9], scalar1=-1.0, scalar2=None, op0=ALU.mult), ["r1i"], ["r1k"])
        P.op("act", lambda e: e.activation(out=r1[:, 11:12], in_=r1[:, 9:10], func=AF.Exp, bias=r1[:, 10:11]), reads=["r1j", "r1k"], writes=["r1l"])
        dvo(lambda e: e.tensor_scalar(out=r1[:, 12:13], in0=r1[:, 11:12], scalar1=1.0, scalar2=None, op0=ALU.add), ["r1l"], ["r1m"])
        dvo(lambda e: e.reciprocal(out=r1[:, 13:14], in_=r1[:, 12:13]), ["r1m"], ["r1n"])
        dvo(lambda e, ti=ti: e.tensor_tensor(out=wk[:, ti, 0:1], in0=r1[:, 7:8], in1=r1[:, 13:14], op=ALU.mult), ["r1h", "r1n"], ["wk"])
        dvo(lambda e, ti=ti: e.tensor_tensor(out=wk[:, ti, 1:2], in0=wk[:, ti, 0:1], in1=r1[:, 11:12], op=ALU.mult), ["wk", "r1l"], ["wk"])
        for g in range(8):
            dvo(lambda e, g=g, ti=ti: e.tensor_scalar(out=OH1[:, ti, g * 8:(g + 1) * 8], in0=oh1, scalar1=ohg[:, g:g + 1], scalar2=None, op0=ALU.mult), ["oh1", "ohg"], ["OH1"])
            dvo(lambda e, g=g, ti=ti: e.tensor_scalar(out=OH2[:, ti, g * 8:(g + 1) * 8], in0=oh2, scalar1=ohg[:, g:g + 1], scalar2=None, op0=ALU.mult), ["oh2", "ohg"], ["OH2"])
        dvo(lambda e, ti=ti: e.tensor_tensor(out=OHs[:, ti, :], in0=OH1[:, ti, :], in1=OH2[:, ti, :], op=ALU.add), ["OH1", "OH2"], ["OHs"])
        for j in range(ti):
            P.op("pe", lambda e, j=j: e.matmul(psum[:, 1, 0:64], lhsT=ones, rhs=OHs[:, j, :], start=(j == 0), stop=False), reads=["OHs", "cst"], writes=[("ps", 1)])
        P.op("pe", lambda e, ti=ti: e.matmul(psum[:, 1, 0:64], lhsT=Lstrict, rhs=OHs[:, ti, :], start=(ti == 0), stop=True), reads=["OHs", "cst2"], writes=[("ps", 1)])
        P.op("act", lambda e, ti=ti: e.activation(out=rank[:, ti, :], in_=psum[:, 1, 0:64], func=AF.Copy), reads=[("ps", 1)], writes=["rank"])
        dvo(lambda e, ti=ti: e.tensor_tensor(out=tmp64, in0=rank[:, ti, :], in1=erow256, op=ALU.add), ["rank", "cst2"], ["tmp64"])
        for k, OHk in enumerate((OH1, OH2)):
            dvo(lambda e, ti=ti, OHk=OHk: e.tensor_tensor(out=lg[:, 0:64], in0=tmp64, in1=OHk[:, ti, :], op=ALU.mult), ["tmp64", "OH1", "OH2"], ["lg"])
            dvo(lambda e, ti=ti, k=k: e.tensor_reduce(out=idxf[:, ti, k:k + 1], in_=lg[:, 0:64], axis=AX.X, op=ALU.add), ["lg"], ["idxf"])
    P.op("dve", lambda e: e.tensor_copy(out=idxi, in_=idxf), reads=["idxf"], writes=["idxi"])
    P.barrier()
    if debug == 5:
        for ti in range(8):
            P.dma("sp", lambda e, ti=ti: e.dma_start(out=dbg[0:128, ti * 64:(ti + 1) * 64], in_=OH1[:, ti, :]), writes=["dbg"])
            P.dma("sp", lambda e, ti=ti: e.dma_start(out=dbg[0:128, 512 + ti * 64:512 + (ti + 1) * 64], in_=OH2[:, ti, :]), writes=["dbg"])
            P.dma("sp", lambda e, ti=ti: e.dma_start(out=dbg[0:128, 1024 + ti * 64:1024 + (ti + 1) * 64], in_=rank[:, ti, :]), writes=["dbg"])
        P.dma("sp", lambda e: e.dma_start(out=dbg[0:128, 1536:1552], in_=wk.rearrange("p t n -> p (t n)")), writes=["dbg"])
        P.dma("sp", lambda e: e.dma_start(out=dbg[0:128, 1552:1568], in_=idxf.rearrange("p t n -> p (t n)")), writes=["dbg"])
        P.barrier()
    egv = eg_d.rearrange("(e k p) n -> e p k n", p=128, k=16)
    euv = eu_d.rearrange("(e k p) n -> e p k n", p=128, k=16)
    edv = ed_d.rearrange("(e f p) n -> e p f n", p=128, f=8)
    cnt = [0, 0]
    for ex in range(nexp):
        for ti in range(8):
            P.op("dve", lambda e, ti=ti, ex=ex: e.tensor_scalar(out=Sel[:, ti, :], in0=iota256[:, 0:CAP], scalar1=rank[:, ti, ex:ex + 1], scalar2=OHs[:, ti, ex:ex + 1],
                                                             op0=ALU.is_equal, op1=ALU.mult), reads=["rank", "OHs", "cst2"], writes=[("Sel", ti)])
        for kc in range(16):
            bank = kc % 2
            for ti in range(8):
                P.op("pe", lambda e, kc=kc, ti=ti, bank=bank: e.matmul(psum[:, bank, 0:CAP], lhsT=u2buf[:, ti, kc * 128:(kc + 1) * 128], rhs=Sel[:, ti, :],
                                                                       start=(ti == 0), stop=(ti == 7)), reads=[("u2", ti), ("Sel", ti)], writes=[("ps", bank)])
            P.op("act" if kc % 2 else "dve",
                 (lambda e, kc=kc, bank=bank: e.activation(out=xbT[:, kc, :], in_=psum[:, bank, 0:CAP], func=AF.Copy)) if kc % 2 else
                 (lambda e, kc=kc, bank=bank: e.tensor_copy(out=xbT[:, kc, :], in_=psum[:, bank, 0:CAP])),
                 reads=[("ps", bank)], writes=[("xbT", kc)])
        for fc in range(8):
            pb = cnt[0] % 2
            cnt[0] += 1
            P.dma("sp", lambda e, ex=ex, fc=fc, pb=pb: e.dma_start(out=wgb[pb], in_=egv[ex, :, :, fc * 128:(fc + 1) * 128]), writes=[("wgb", pb)])
            P.dma("sp", lambda e, ex=ex, fc=fc, pb=pb: e.dma_start(out=wub[pb], in_=euv[ex, :, :, fc * 128:(fc + 1) * 128]), writes=[("wub", pb)])
            bg = 2 + 2 * (fc % 2)
            for kc in range(16):
                P.op("pe", lambda e, kc=kc, pb=pb, bg=bg: e.matmul(psum[:, bg, 0:CAP], lhsT=wgb[pb][:, kc, :], rhs=xbT[:, kc, :], start=(kc == 0), stop=(kc == 15)),
                     reads=[("wgb", pb), ("xbT", kc)], writes=[("ps", bg)])
            for kc in range(16):
                P.op("pe", lambda e, kc=kc, pb=pb, bg=bg: e.matmul(psum[:, bg + 1, 0:CAP], lhsT=wub[pb][:, kc, :], rhs=xbT[:, kc, :], start=(kc == 0), stop=(kc == 15)),
                     reads=[("wub", pb), ("xbT", kc)], writes=[("ps", bg + 1)])
            P.op("act", lambda e, fc=fc, bg=bg: e.activation(out=h1T[:, fc, :], in_=psum[:, bg, 0:CAP], func=AF.Silu), reads=[("ps", bg)], writes=[("h1T", fc)])
            P.op("dve", lambda e, fc=fc, bg=bg: e.tensor_tensor(out=h1T[:, fc, :], in0=psum[:, bg + 1, 0:CAP], in1=h1T[:, fc, :], op=ALU.mult),
                 reads=[("ps", bg + 1), ("h1T", fc)], writes=[("h1T", fc)])
        for db in range(4):
            pb = cnt[1] % 2
            cnt[1] += 1
            P.dma("sp", lambda e, ex=ex, db=db, pb=pb: e.dma_start(out=wdb[pb], in_=edv[ex, :, :, db * 512:(db + 1) * 512]), writes=[("wdb", pb)])
            for sb in range(CAP // 128):
                bank = 6 + sb
                for fc in range(8):
                    P.op("pe", lambda e, fc=fc, sb=sb, pb=pb, bank=bank: e.matmul(psum[:, bank, :], lhsT=h1T[:, fc, sb * 128:(sb + 1) * 128], rhs=wdb[pb][:, fc, :],
                                                                                 start=(fc == 0), stop=(fc == 7)), reads=[("h1T", fc), ("wdb", pb)], writes=[("ps", bank)])
                ys = yst[sb]
                P.op("act" if sb else "dve",
                     (lambda e, ys=ys, bank=bank: e.activation(out=ys, in_=psum[:, bank, :], func=AF.Copy)) if sb else
                     (lambda e, ys=ys, bank=bank: e.tensor_copy(out=ys, in_=psum[:, bank, :])),
                     reads=[("ps", bank)], writes=[("yst", sb)])
                r0 = ex * CAP + sb * 128
                P.dma("pool", lambda e, ys=ys, r0=r0, db=db: e.dma_start(out=YSL[r0:r0 + 128, db * 512:(db + 1) * 512], in_=ys), reads=[("yst", sb)], writes=[("dram", "YSL")])
    P.barrier()
    bcast_rows([("gt2", gt2o)])
    fgb_t = wdb[0].rearrange("p f n -> p (f n)")[:, 0:2048]
    P.dma("sp", lambda e: e.dma_start(out=fgb_t, in_=fg_d), writes=["fgb"])
    y1 = wgb[1].rearrange("p k n -> p (k n)")
    y2 = wub[1].rearrange("p k n -> p (k n)")
    for ti in range(8):
        P.dma("sp", lambda e, ti=ti: e.dma_start(out=hti, in_=HSCR[ti * 128:(ti + 1) * 128, :]), writes=["hti"])
        P.dma("pool", lambda e, ti=ti: e.indirect_dma_start(out=y1, out_offset=None, in_=YSL, in_offset=bass.IndirectOffsetOnAxis(ap=idxi[:, ti, 0:1], axis=0)),
              reads=[("dram", "YSL"), "idxi"], writes=["y1"])
        P.dma("pool", lambda e, ti=ti: e.indirect_dma_start(out=y2, out_offset=None, in_=YSL, in_offset=bass.IndirectOffsetOnAxis(ap=idxi[:, ti, 1:2], axis=0)),
              reads=[("dram", "YSL"), "idxi"], writes=["y2"])
        P.op("dve", lambda e, ti=ti: e.tensor_scalar(out=y1, in0=y1, scalar1=wk[:, ti, 0:1], scalar2=None, op0=ALU.mult), reads=["y1", "wk"], writes=["y1"])
        P.op("dve", lambda e, ti=ti: e.scalar_tensor_tensor(out=y1, in0=y2, scalar=wk[:, ti, 1:2], in1=y1, op0=ALU.mult, op1=ALU.add), reads=["y1", "y2", "wk"], writes=["y1"])
        P.op("dve", lambda e: e.tensor_tensor(out=y1, in0=y1, in1=bc["gt2"], op=ALU.mult), reads=["y1", "bcgt2"], writes=["y1"])
        P.op("dve", lambda e: e.tensor_tensor(out=y1, in0=y1, in1=hti, op=ALU.add), reads=["y1", "hti"], writes=["y1"])
        P.op("act", lambda e: e.activation(out=u2T, in_=y1, func=AF.Square, accum_out=r1[:, 0:1]), reads=["y1"], writes=[("u2T", 0), ("u2T", 1), ("u2T", 2), ("u2T", 3), "r1a"])
        P.op("dve", lambda e: e.tensor_scalar(out=r1[:, 1:2], in0=r1[:, 0:1], scalar1=1.0 / D, scalar2=1e-6, op0=ALU.mult, op1=ALU.add), reads=["r1a"], writes=["r1b"])
        P.op("act", lambda e: e.activation(out=r1[:, 2:3], in_=r1[:, 1:2], func=AF.Sqrt), reads=["r1b"], writes=["r1c"])
        P.op("dve", lambda e: e.reciprocal(out=r1[:, 3:4], in_=r1[:, 2:3]), reads=["r1c"], writes=["r1d"])
        P.op("dve", lambda e: e.scalar_tensor_tensor(out=y2, in0=y1, scalar=r1[:, 3:4], in1=fgb_t, op0=ALU.mult, op1=ALU.mult), reads=["y1", "r1d", "fgb"], writes=["y2"])
        P.dma("sp", lambda e, ti=ti: e.dma_start(out=out[ti * 128:(ti + 1) * 128, :], in_=y2), reads=["y2"], writes=["out"])
    P.barrier()
    P.emit()
    es.close()
    return nc


def make_consts():
    c = np.zeros((128, 2560), np.float32)
    c[:, 1024:1536] = 1.0
    c[:, 1024:1536:64] = 0.0
    su = np.triu(np.ones((64, 64), np.float32), 1)
    iu = np.triu(np.ones((64, 64), np.float32), 0)
    for i in range(4):
        c[0:64, 1536 + i * 128:1536 + i * 128 + 64] = su
        c[0:64, 1536 + i * 128 + 64:1536 + (i + 1) * 128] = iu
        c[0:64, 2048 + i * 64:2048 + (i + 1) * 64] = su.T
        c[0:64, 2304 + i * 64:2304 + (i + 1) * 64] = np.eye(64, dtype=np.float32)
    c[:, 0:128] = np.eye(128, dtype=np.float32)
    c[:, 128:256] = 1.0
    c[0:64, 256:320] = 1.0
    c[64:128, 320:384] = 1.0
    R = np.zeros((128, 128), np.float32)
    for blk in range(2):
        for i in range(8):
            R[blk * 64 + i, blk * 64 + i + 8] = -1.0
            R[blk * 64 + i + 8, blk * 64 + i] = 1.0
    c[:, 384:512] = R.T
    kk, qq = np.meshgrid(np.arange(128), np.arange(128), indexing="ij")
    c[:, 512:640] = (kk <= qq).astype(np.float32)
    inv = np.float32(500000.0) ** (-np.arange(8, dtype=np.float32) / np.float32(8))
    for blk in range(2):
        for i in range(16):
            c[blk * 64 + i, 640] = inv[i % 8]
    return c


def make_consts2():
    c = np.zeros((128, 512), np.float32)
    a, b = np.meshgrid(np.arange(128), np.arange(128), indexing="ij")
    c[:, 0:128] = (a < b).astype(np.float32)
    c[:, 128:384] = np.arange(256, dtype=np.float32)[None, :]
    c[:, 384:448] = (np.arange(64, dtype=np.float32) * 256.0)[None, :]
    return c


def kernel(**inp):
    debug = int(inp.pop("_debug", 0))
    f = lambda a: np.ascontiguousarray(np.asarray(a, dtype=np.float32))
    x = f(inp["x"]).reshape(NT, D)
    c = f(inp["c"])
    cT = np.ascontiguousarray(c.T.reshape(KC, 128, 2).transpose(1, 0, 2))
    ada_w = f(inp["ada_w"])[0]
    ada_b = f(inp["ada_b"])[0]
    ada_bT = np.ascontiguousarray(np.repeat(ada_b.reshape(96, 128).T[:, :, None], 2, axis=2))
    g1T = np.ascontiguousarray(f(inp["norm_mix_g"])[0].reshape(KC, 128).T)
    w_in = f(inp["w_in"])[0]
    consts = make_consts()
    posi = np.ascontiguousarray(np.asarray(inp["positions"], dtype=np.int32))
    lamp = np.ascontiguousarray(np.broadcast_to(np.concatenate(
        [f(inp["lambda_q1"])[0], f(inp["lambda_k1"])[0], f(inp["lambda_q2"])[0], f(inp["lambda_k2"])[0]])[None, :], (128, 256)))
    sublng = np.ascontiguousarray(f(inp["subln_g"])[0].reshape(128, 1))
    mu = f(inp["tshift_mu"])[0]
    consts2 = make_consts2()
    wupa = f(inp["w_up_attn"])[0]
    wupb = f(inp["w_up_rwkv"])[0]
    wgt = np.ascontiguousarray(w_in[:, 6400:10496])
    wout = f(inp["w_out"])[0]
    g2n = np.ascontiguousarray(np.broadcast_to(f(inp["norm_ffn_g"])[0][None, :], (128, D)))
    fgb = np.ascontiguousarray(np.broadcast_to(f(inp["final_g"])[None, :], (128, D)))
    wr = np.ascontiguousarray(np.concatenate([f(inp["w_route_group"])[0], f(inp["w_route_expert"])[0]], axis=1))
    br = np.ascontiguousarray(np.broadcast_to(np.concatenate([f(inp["b_route_group"])[0], f(inp["b_route_expert"])[0]])[None, :], (128, 72)))
    eg = f(inp["w_gate"])[0].reshape(64 * D, 1024)
    eu = f(inp["w_up"])[0].reshape(64 * D, 1024)
    ed = f(inp["w_down"])[0].reshape(64 * 1024, D)
    in_maps = []
    for cid in range(NCORES):
        cs = slice(cid * 128, (cid + 1) * 128)
        mu5 = np.stack([mu[0:1024][cs], mu[1024:2048][cs], mu[2048:3072][cs], mu[3072:3200], mu[3200:3328]], axis=1)
        rwv = np.stack([f(inp["w0"])[0][cs], f(inp["a0"])[0][cs], f(inp["k_k"])[0][cs], f(inp["k_a"])[0][cs],
                        f(inp["lnx_g"])[0][cs], f(inp["lnx_b"])[0][cs], f(inp["r_k"])[0].reshape(-1)[cs], np.zeros(128, np.float32)], axis=1)
        wa2 = np.concatenate([f(inp["w2"])[0][:, cs], f(inp["a2"])[0][:, cs]], axis=0)
        g2s = f(inp["g2"])[0][:, cs]
        bsel = np.zeros((128, 2), np.float32)
        bsel[:, cid // 4] = 1.0
        pp = np.arange(128)[:, None]
        kk_ = (np.arange(32) // 2)[None, :]
        ob_ = (np.arange(32) % 2)[None, :]
        yidx = (((kk_ % 8) * 256 + (kk_ // 8) * 128 + pp) * 16 + cid * 2 + ob_).astype(np.int32)
        wst1 = np.concatenate([
            w_in[:, 0:1024][:, cs], w_in[:, 1024:2048][:, cs], w_in[:, 2048:3072][:, cs],
            w_in[:, 3072:4096][:, cs], w_in[:, 4096:5120][:, cs], w_in[:, 5120:6144][:, cs],
            w_in[:, 6144:6272], w_in[:, 6272:6400]], axis=1)
        in_maps.append(dict(x=x, cT=cT, ada_w=ada_w, ada_b=ada_bT, g1T=g1T, wst1=np.ascontiguousarray(wst1),
                            consts=consts, posi=posi, lamp=lamp, sublng=sublng,
                            mu5=np.ascontiguousarray(mu5), rwv=np.ascontiguousarray(rwv), wa2=np.ascontiguousarray(wa2),
                            g2s=np.ascontiguousarray(g2s), x_own=np.ascontiguousarray(x[cid * OWN:(cid + 1) * OWN]), bsel=bsel,
                            wupa=wupa, wupb=wupb, wgt=wgt, wout=wout, g2n=g2n, fgb=fgb, wr=wr, br=br,
                            consts2=consts2))
    import os
    KBLK = int(os.environ.get('KBLK', '16')); KBLKA = int(os.environ.get('KBLKA', '24'))
    NEXP = int(os.environ.get('NEXP', '64'))
    keys1 = ["x", "cT", "ada_w", "ada_b", "g1T", "wst1", "consts", "posi", "lamp", "sublng", "mu5", "rwv", "wa2", "g2s"]
    maps1 = [{k: m[k] for k in keys1} for m in in_maps]
    if KBLK < 16 or KBLKA < 24:
        for m in maps1:
            m['x'] = np.ascontiguousarray(m['x'][:KBLK * 512]); m['ada_w'] = np.ascontiguousarray(m['ada_w'][:, :512 * KBLKA])
    nc = build(debug if debug < 4 else 0)
    res = run_bass_kernel_spmd(nc, maps1, core_ids=list(range(NCORES)))
    if 0 < debug < 4:
        return [r["dbg"] for r in res.results]
    ysrc = [r["ysrc"] for r in res.results]
    auxs = [r["aux"] for r in res.results]
    keys2 = ["consts", "consts2", "x_own", "bsel", "wupa", "wupb", "wgt", "wout", "g2n", "fgb", "wr", "br"]
    maps2 = []
    for cid in range(NCORES):
        m = {k: in_maps[cid][k] for k in keys2}
        oc = slice(cid * OWN, (cid + 1) * OWN)
        m["yown"] = np.ascontiguousarray(np.stack([ysrc[kc % 8][(kc // 8) * 128:(kc // 8 + 1) * 128, oc] for kc in range(16)], axis=0))
        m["aux_in"] = auxs[cid]
        m["eg"] = eg[:NEXP * D]; m["eu"] = eu[:NEXP * D]; m["ed"] = ed[:NEXP * 1024]
        maps2.append(m)
    nc2 = build2(debug if debug >= 4 else 0, NEXP)
    res2 = run_bass_kernel_spmd(nc2, maps2, core_ids=list(range(NCORES)))
    if debug >= 4:
        return [r["dbg"] for r in res2.results] + [r["out"] for r in res2.results]
    outp = np.concatenate([r["out"] for r in res2.results], axis=0)
    return outp.reshape(2, S, D).astype(np.float32)
```
